# Optimizing a Trainium2 kernel written in Bass

```python
import math
import jax, jax.numpy as jnp
from jax import lax
import numpy as np

D_MODEL = 2048
BATCH = 16
SEQ = 2048
DEPTH = 4

N_MIXERS = 4
N_SWA = (DEPTH + 3) // N_MIXERS
N_RWKV = (DEPTH + 2) // N_MIXERS
N_MLSTM = (DEPTH + 1) // N_MIXERS
N_FOX = DEPTH // N_MIXERS

SWA_HEAD_DIM = 64
SWA_HEADS = D_MODEL // SWA_HEAD_DIM
SWA_KV_HEADS = SWA_HEADS // 8
SWA_GROUP = SWA_HEADS // SWA_KV_HEADS
SWA_WIDTH = SWA_HEADS * SWA_HEAD_DIM
SWA_KV_WIDTH = SWA_KV_HEADS * SWA_HEAD_DIM
SWA_IN = 2 * SWA_WIDTH + 2 * SWA_KV_WIDTH
SWA_WINDOW = 128
SWA_BLOCK = 128
REL_BUCKETS = 32
REL_MAX_DIST = 128
RWKV_HEAD_DIM = 64
RWKV_HEADS = D_MODEL // RWKV_HEAD_DIM
RWKV_DECAY_LORA = 96
RWKV_AAA_LORA = 96
RWKV_N_MIX = 6
MLSTM_HEADS = 8
MLSTM_V_DIM = D_MODEL // MLSTM_HEADS
MLSTM_QK_DIM = MLSTM_V_DIM // 2
MLSTM_QK_WIDTH = MLSTM_HEADS * MLSTM_QK_DIM
MLSTM_V_WIDTH = MLSTM_HEADS * MLSTM_V_DIM
MLSTM_IN = 2 * MLSTM_QK_WIDTH + 3 * MLSTM_V_WIDTH + 2 * MLSTM_HEADS
MLSTM_CHUNK = 128
FOX_HEAD_DIM = 64
FOX_HEADS = D_MODEL // FOX_HEAD_DIM
FOX_WIDTH = FOX_HEADS * FOX_HEAD_DIM
FOX_IN = 4 * FOX_WIDTH + FOX_HEADS
FOX_BLOCK = 128

RMS_EPS = 1e-6
GN_EPS = 64e-5

kernel_name = "hybrid_interleaved_swa_rwkv7_mlstm_fox"


def rms_norm(x, g):
    xf = x.astype(jnp.float32)
    y = xf * lax.rsqrt(jnp.mean(xf * xf, axis=-1, keepdims=True) + RMS_EPS)
    return (y * g.astype(jnp.float32)).astype(x.dtype)


def t5_bucket(dist):
    max_exact = REL_BUCKETS // 2
    d_f = jnp.maximum(dist, 1).astype(jnp.float32)
    large = max_exact + (jnp.log(d_f / max_exact) / math.log(REL_MAX_DIST / max_exact)
                         * (REL_BUCKETS - max_exact)).astype(jnp.int32)
    large = jnp.minimum(large, REL_BUCKETS - 1)
    return jnp.where(dist < max_exact, dist, large)


def swa_sink_mixer(h, w_in, q_gain, k_gain, sinks, rel_table, w_out):
    bsz, seq, _ = h.shape
    L = SWA_BLOCK
    nb = seq // L
    q, k, v, gate = jnp.split(h @ w_in, [SWA_WIDTH, SWA_WIDTH + SWA_KV_WIDTH,
                                         SWA_WIDTH + 2 * SWA_KV_WIDTH], axis=-1)
    q = rms_norm(q.reshape(bsz, seq, SWA_KV_HEADS, SWA_GROUP, SWA_HEAD_DIM), q_gain)
    k = rms_norm(k.reshape(bsz, seq, SWA_KV_HEADS, SWA_HEAD_DIM), k_gain)
    v = v.reshape(bsz, seq, SWA_KV_HEADS, SWA_HEAD_DIM)
    pad = ((0, 0), (L, 0), (0, 0), (0, 0))
    k_pad = jnp.pad(k, pad)
    v_pad = jnp.pad(v, pad)
    qi = jnp.arange(L)[:, None]
    kj = jnp.arange(2 * L)[None, :]
    dist = qi + L - kj
    in_band = (dist >= 0) & (dist < SWA_WINDOW)
    bias = rel_table.astype(jnp.float32)[t5_bucket(jnp.maximum(dist, 0))]
    bias = bias.transpose(2, 0, 1).reshape(SWA_KV_HEADS, SWA_GROUP, L, 2 * L)
    sink = sinks.astype(jnp.float32).reshape(SWA_KV_HEADS, SWA_GROUP, 1, 1)
    scale = SWA_HEAD_DIM ** -0.5
    q_blocks = q.reshape(bsz, nb, L, SWA_KV_HEADS, SWA_GROUP, SWA_HEAD_DIM).transpose(1, 0, 2, 3, 4, 5)

    def one_block(args):
        j, qj = args
        start = j * L
        kb = lax.dynamic_slice_in_dim(k_pad, start, 2 * L, axis=1)
        vb = lax.dynamic_slice_in_dim(v_pad, start, 2 * L, axis=1)
        s = jnp.einsum('bqhgd,bkhd->bhgqk', qj, kb).astype(jnp.float32) * scale + bias
        valid = in_band & (start - L + kj >= 0)
        s = jnp.where(valid, s, -jnp.inf)
        m = jnp.maximum(jnp.max(s, axis=-1, keepdims=True), sink)
        p = jnp.exp(s - m)
        p = p / (jnp.sum(p, axis=-1, keepdims=True) + jnp.exp(sink - m))
        return jnp.einsum('bhgqk,bkhd->bqhgd', p.astype(vb.dtype), vb)

    o = lax.map(one_block, (jnp.arange(nb), q_blocks))
    o = o.transpose(1, 0, 2, 3, 4, 5).reshape(bsz, seq, SWA_WIDTH)
    return (o * jax.nn.silu(gate)) @ w_out


def rwkv7_mixer(h, mix, w_in, w0, w_lora1, w_lora2, a0, a_lora1, a_lora2,
                k_k, k_a, r_k, gn_w, gn_b, w_out):
    bsz, seq, d = h.shape
    H, N = RWKV_HEADS, RWKV_HEAD_DIM
    f32 = jnp.float32
    xx = jnp.pad(h, ((0, 0), (1, 0), (0, 0)))[:, :-1] - h
    xm = h[None] + xx[None] * mix[:, None, None, :]
    r, k, v, g = jnp.einsum('cbsd,cde->cbse', xm[:4], w_in)
    w_pre = (w0 + jnp.tanh(xm[4] @ w_lora1) @ w_lora2).astype(f32)
    log_w = -jnp.exp(-jax.nn.softplus(-w_pre) - 0.5)
    a = jax.nn.sigmoid((a0 + (xm[5] @ a_lora1) @ a_lora2).astype(f32))

    def heads(t):
        return t.astype(f32).reshape(bsz, seq, H, N)

    r, k, v, a, log_w = heads(r), heads(k), heads(v), heads(a), heads(log_w)
    kk = k * k_k.astype(f32).reshape(H, N)
    kk = kk / jnp.maximum(jnp.sqrt(jnp.sum(kk * kk, axis=-1, keepdims=True)), 1e-12)
    k = k * (1.0 + (a - 1.0) * k_a.astype(f32).reshape(H, N))

    def step(state, inp):
        r_t, w_t, k_t, v_t, kk_t, a_t = inp
        sa = jnp.einsum('bhvk,bhk->bhv', state, -kk_t)
        state = (state * w_t[:, :, None, :]
                 + sa[..., None] * (kk_t * a_t)[:, :, None, :]
                 + v_t[..., None] * k_t[:, :, None, :])
        return state, jnp.einsum('bhvk,bhk->bhv', state, r_t)

    def tm(t):
        return t.transpose(1, 0, 2, 3)

    state0 = jnp.zeros((bsz, H, N, N), f32)
    _, y = lax.scan(step, state0, (tm(r), tm(jnp.exp(log_w)), tm(k), tm(v), tm(kk), tm(a)))
    y = tm(y)
    mu = jnp.mean(y, axis=-1, keepdims=True)
    var = jnp.mean(jnp.square(y - mu), axis=-1, keepdims=True)
    y = ((y - mu) * lax.rsqrt(var + GN_EPS)).reshape(bsz, seq, d) * gn_w + gn_b
    bonus = jnp.sum(r * k * r_k.astype(f32), axis=-1, keepdims=True) * v
    y = y + bonus.reshape(bsz, seq, d)
    return (y.astype(h.dtype) * jax.nn.silu(g)) @ w_out


def mlstm_mixer(h, w_in, b_i, b_f, h_gain, w_out):
    bsz, seq, _ = h.shape
    H, dk, dv, L = MLSTM_HEADS, MLSTM_QK_DIM, MLSTM_V_DIM, MLSTM_CHUNK
    nc = seq // L
    f32 = jnp.float32
    s1 = MLSTM_QK_WIDTH
    s2 = s1 + MLSTM_QK_WIDTH
    s3 = s2 + MLSTM_V_WIDTH
    s4 = s3 + MLSTM_V_WIDTH
    s5 = s4 + MLSTM_V_WIDTH
    s6 = s5 + H
    q, k, v, o_pre, gate, i_pre, f_pre = jnp.split(h @ w_in, [s1, s2, s3, s4, s5, s6], axis=-1)
    q = q.astype(f32).reshape(bsz, seq, H, dk) * dk ** -0.5
    k = k.astype(f32).reshape(bsz, seq, H, dk)
    v = v.astype(f32).reshape(bsz, seq, H, dv)
    i_log = (i_pre + b_i).astype(f32)
    f_log = jax.nn.log_sigmoid((f_pre + b_f).astype(f32))

    def chunks(t):
        t = t.reshape((bsz, nc, L) + t.shape[2:])
        return jnp.moveaxis(t, (1, 3), (0, 2))

    tri = jnp.tril(jnp.ones((L, L), dtype=bool))

    def chunk_step(carry, inp):
        C, n, m = carry
        qc, kc, vc, ic, fc = inp
        b = jnp.cumsum(fc, axis=-1)
        g = b[..., -1]
        dmat = jnp.where(tri, b[..., :, None] - b[..., None, :] + ic[..., None, :], -jnp.inf)
        inter = b + m[..., None]
        m_t = jnp.maximum(inter, jnp.max(dmat, axis=-1))
        w_intra = jnp.exp(dmat - m_t[..., None])
        w_inter = jnp.exp(inter - m_t)
        sw = jnp.einsum('bhtd,bhsd->bhts', qc, kc) * w_intra
        num = (w_inter[..., None] * jnp.einsum('bhvk,bhtk->bhtv', C, qc)
               + jnp.einsum('bhts,bhsv->bhtv', sw, vc))
        den = w_inter * jnp.einsum('bhk,bhtk->bht', n, qc) + jnp.sum(sw, axis=-1)
        h_t = num / jnp.maximum(jnp.abs(den), jnp.exp(-m_t))[..., None]
        decay_s = g[..., None] - b + ic
        m_new = jnp.maximum(g + m, jnp.max(decay_s, axis=-1))
        ws = jnp.exp(decay_s - m_new[..., None])
        carry_scale = jnp.exp(g + m - m_new)
        C = carry_scale[..., None, None] * C + jnp.einsum('bhs,bhsv,bhsk->bhvk', ws, vc, kc)
        n = carry_scale[..., None] * n + jnp.einsum('bhs,bhsk->bhk', ws, kc)
        return (C, n, m_new), h_t

    carry0 = (jnp.zeros((bsz, H, dv, dk), f32), jnp.zeros((bsz, H, dk), f32), jnp.zeros((bsz, H), f32))
    _, hs = lax.scan(chunk_step, carry0, (chunks(q), chunks(k), chunks(v), chunks(i_log), chunks(f_log)))
    hs = jnp.moveaxis(hs, (0, 2), (1, 3)).reshape(bsz, seq, H, dv)
    hs = rms_norm(hs, h_gain).reshape(bsz, seq, MLSTM_V_WIDTH).astype(h.dtype)
    return (hs * jax.nn.sigmoid(o_pre) * jax.nn.silu(gate)) @ w_out


def fox_mixer(h, w_in, b_f, q_gain, k_gain, w_out):
    bsz, seq, _ = h.shape
    H, dh, L = FOX_HEADS, FOX_HEAD_DIM, FOX_BLOCK
    nb = seq // L
    W = FOX_WIDTH
    q, k, v, gate, f_pre = jnp.split(h @ w_in, [W, 2 * W, 3 * W, 4 * W], axis=-1)
    q = rms_norm(q.reshape(bsz, seq, H, dh), q_gain)
    k = rms_norm(k.reshape(bsz, seq, H, dh), k_gain)
    v = v.reshape(bsz, seq, H, dh)
    log_f = jax.nn.log_sigmoid((f_pre + b_f).astype(jnp.float32))
    F = jnp.cumsum(log_f, axis=1).transpose(0, 2, 1)
    q_blocks = q.reshape(bsz, nb, L, H, dh).transpose(1, 0, 2, 3, 4)
    F_blocks = F.reshape(bsz, H, nb, L).transpose(2, 0, 1, 3)
    key_pos = jnp.arange(seq)
    scale = dh ** -0.5

    def one_block(args):
        j, qj, Fj = args
        s = (jnp.einsum('bqhd,bkhd->bhqk', qj, k).astype(jnp.float32) * scale
             + (Fj[..., :, None] - F[:, :, None, :]))
        q_pos = j * L + jnp.arange(L)
        s = jnp.where(key_pos[None, :] <= q_pos[:, None], s, -jnp.inf)
        p = jax.nn.softmax(s, axis=-1)
        return jnp.einsum('bhqk,bkhd->bqhd', p.astype(v.dtype), v)

    o = lax.map(one_block, (jnp.arange(nb), q_blocks, F_blocks))
    o = o.transpose(1, 0, 2, 3, 4).reshape(bsz, seq, W)
    return (o * jax.nn.silu(gate)) @ w_out


def setup_inputs(seed: int = 0) -> dict:
    key = jax.random.key(seed)
    keys = jax.random.split(key, 48)
    counter = [0]

    def next_key():
        kk = keys[counter[0]]
        counter[0] += 1
        return kk

    def nrm(shape, scale):
        return jax.random.normal(next_key(), shape, jnp.float32) * scale

    def gain(shape):
        return 1.0 + nrm(shape, 0.05)

    D = D_MODEL
    return {
        "x": nrm((BATCH, SEQ, D), 1.0),
        "rel_bias": nrm((REL_BUCKETS, SWA_HEADS), 0.2),
        "swa_norm": gain((N_SWA, D)),
        "swa_w_in": nrm((N_SWA, D, SWA_IN), D ** -0.5),
        "swa_q_gain": gain((N_SWA, SWA_HEAD_DIM)),
        "swa_k_gain": gain((N_SWA, SWA_HEAD_DIM)),
        "swa_sinks": nrm((N_SWA, SWA_HEADS), 0.5),
        "swa_w_out": nrm((N_SWA, SWA_WIDTH, D), SWA_WIDTH ** -0.5),
        "rwkv_norm": gain((N_RWKV, D)),
        "rwkv_mix": jax.random.uniform(next_key(), (N_RWKV, RWKV_N_MIX, D), jnp.float32),
        "rwkv_w_in": nrm((N_RWKV, 4, D, D), D ** -0.5),
        "rwkv_w0": -2.0 + nrm((N_RWKV, D), 1.0),
        "rwkv_w_lora1": nrm((N_RWKV, D, RWKV_DECAY_LORA), D ** -0.5),
        "rwkv_w_lora2": nrm((N_RWKV, RWKV_DECAY_LORA, D), 0.5 * RWKV_DECAY_LORA ** -0.5),
        "rwkv_a0": nrm((N_RWKV, D), 0.1),
        "rwkv_a_lora1": nrm((N_RWKV, D, RWKV_AAA_LORA), D ** -0.5),
        "rwkv_a_lora2": nrm((N_RWKV, RWKV_AAA_LORA, D), 0.5 * RWKV_AAA_LORA ** -0.5),
        "rwkv_k_k": 0.85 + nrm((N_RWKV, D), 0.05),
        "rwkv_k_a": 1.0 + nrm((N_RWKV, D), 0.05),
        "rwkv_r_k": nrm((N_RWKV, RWKV_HEADS, RWKV_HEAD_DIM), 0.1),
        "rwkv_gn_w": gain((N_RWKV, D)),
        "rwkv_gn_b": nrm((N_RWKV, D), 0.02),
        "rwkv_w_out": nrm((N_RWKV, D, D), D ** -0.5),
        "mlstm_norm": gain((N_MLSTM, D)),
        "mlstm_w_in": nrm((N_MLSTM, D, MLSTM_IN), D ** -0.5),
        "mlstm_b_i": nrm((N_MLSTM, MLSTM_HEADS), 0.1),
        "mlstm_b_f": 3.0 + nrm((N_MLSTM, MLSTM_HEADS), 0.5),
        "mlstm_h_gain": gain((N_MLSTM, MLSTM_HEADS, MLSTM_V_DIM)),
        "mlstm_w_out": nrm((N_MLSTM, MLSTM_V_WIDTH, D), MLSTM_V_WIDTH ** -0.5),
        "fox_norm": gain((N_FOX, D)),
        "fox_w_in": nrm((N_FOX, D, FOX_IN), D ** -0.5),
        "fox_b_f": 3.0 + nrm((N_FOX, FOX_HEADS), 0.5),
        "fox_q_gain": gain((N_FOX, FOX_HEAD_DIM)),
        "fox_k_gain": gain((N_FOX, FOX_HEAD_DIM)),
        "fox_w_out": nrm((N_FOX, FOX_WIDTH, D), FOX_WIDTH ** -0.5),
    }


def reference(x, rel_bias, swa_norm, swa_w_in, swa_q_gain, swa_k_gain, swa_sinks, swa_w_out,
              rwkv_norm, rwkv_mix, rwkv_w_in, rwkv_w0, rwkv_w_lora1, rwkv_w_lora2, rwkv_a0,
              rwkv_a_lora1, rwkv_a_lora2, rwkv_k_k, rwkv_k_a, rwkv_r_k, rwkv_gn_w, rwkv_gn_b,
              rwkv_w_out, mlstm_norm, mlstm_w_in, mlstm_b_i, mlstm_b_f, mlstm_h_gain, mlstm_w_out,
              fox_norm, fox_w_in, fox_b_f, fox_q_gain, fox_k_gain, fox_w_out):
    for layer in range(DEPTH):
        kind, idx = layer % N_MIXERS, layer // N_MIXERS
        if kind == 0:
            x = x + swa_sink_mixer(rms_norm(x, swa_norm[idx]), swa_w_in[idx], swa_q_gain[idx],
                                   swa_k_gain[idx], swa_sinks[idx], rel_bias, swa_w_out[idx])
        elif kind == 1:
            x = x + rwkv7_mixer(rms_norm(x, rwkv_norm[idx]), rwkv_mix[idx], rwkv_w_in[idx],
                                rwkv_w0[idx], rwkv_w_lora1[idx], rwkv_w_lora2[idx], rwkv_a0[idx],
                                rwkv_a_lora1[idx], rwkv_a_lora2[idx], rwkv_k_k[idx], rwkv_k_a[idx],
                                rwkv_r_k[idx], rwkv_gn_w[idx], rwkv_gn_b[idx], rwkv_w_out[idx])
        elif kind == 2:
            x = x + mlstm_mixer(rms_norm(x, mlstm_norm[idx]), mlstm_w_in[idx], mlstm_b_i[idx],
                                mlstm_b_f[idx], mlstm_h_gain[idx], mlstm_w_out[idx])
        else:
            x = x + fox_mixer(rms_norm(x, fox_norm[idx]), fox_w_in[idx], fox_b_f[idx],
                              fox_q_gain[idx], fox_k_gain[idx], fox_w_out[idx])
    return x
```

```python
import numpy as np
from contextlib import ExitStack
import concourse.bass as bass
import concourse.mybir as mybir
from concourse.bass_utils import run_bass_kernel_spmd

F32 = mybir.dt.float32
BF16 = mybir.dt.bfloat16
AF = mybir.ActivationFunctionType
ALU = mybir.AluOpType
AX = mybir.AxisListType

D = 2048
KC = 16
NEG = -30000.0


class Reg:
    __slots__ = ("name", "w", "r")

    def __init__(self, name=""):
        self.name = name
        self.w = None
        self.r = []


class Op:
    __slots__ = ("eng", "fn", "deps", "idx", "signal", "sig", "is_dma", "key", "ninc")


class Sched:
    def __init__(self, nc):
        self.nc = nc
        self.ops = []
        self.keyregs = {}
        self.keymap = {}

    def add(self, eng, fn, reads=(), writes=(), key=None, ninc=1):
        op = Op()
        op.eng = eng
        op.fn = fn
        op.idx = len(self.ops)
        op.is_dma = key is not None
        op.key = key
        op.ninc = ninc
        op.signal = False
        op.sig = None
        writes = list(writes)
        if key is not None:
            key = self.keymap.setdefault(key, "k%d" % (len(self.keymap) % 64))
            op.key = key
            kr = self.keyregs.get(key)
            if kr is None:
                kr = self.keyregs[key] = Reg(key)
            writes.append(kr)
        raw = set()
        other = set()
        for r in reads:
            if r.w is not None:
                raw.add(r.w)
        for w in writes:
            if w.w is not None:
                other.add(w.w)
            for x in w.r:
                other.add(x)
        for r in reads:
            r.r.append(op.idx)
        for w in writes:
            w.w = op.idx
            w.r = []
        deps = set()
        ops = self.ops
        for d in raw | other:
            if d == op.idx:
                continue
            o = ops[d]
            if o.eng == eng and not o.is_dma and not op.is_dma:
                if eng == "pe":
                    continue
                if d not in raw:
                    continue
            deps.add(d)
        op.deps = deps
        ops.append(op)
        return op

    def barrier(self, engines, scratch_fns):
        marks = []
        for e in engines:
            m = Reg("bar_" + e)
            reads = [kr for kr in self.keyregs.values()] if e == "sp" else []
            self.add(e, scratch_fns[e], reads=reads, writes=[m])
            marks.append(m)
        for e in engines:
            self.add(e, scratch_fns[e], reads=marks, writes=[])

    def emit(self):
        nc = self.nc
        ops = self.ops
        for op in ops:
            for d in op.deps:
                ops[d].signal = True
        cnt = {}
        epoch = {}
        dmacnt = {}
        for op in ops:
            if op.is_dma:
                c = dmacnt.get(op.key, 0) + 16 * op.ninc
                dmacnt[op.key] = c
                op.sig = (("dma", op.key), c)
            elif op.signal:
                e = op.eng
                c = cnt.get(e, 0) + 1
                if c > 30000:
                    epoch[e] = epoch.get(e, 0) + 1
                    c = 1
                cnt[e] = c
                op.sig = ((e, epoch.get(e, 0)), c)
        sems = {}

        def sem(k):
            s = sems.get(k)
            if s is None:
                s = sems[k] = nc.alloc_semaphore("s%d" % len(sems))
            return s

        for op in ops:
            if op.sig is not None:
                sem(op.sig[0])
        per = {}
        for op in ops:
            per.setdefault(op.eng, []).append(op)
        final_dma = dict(dmacnt)

        self.icount = {}

        def run(engname, eng):
            waited = {}
            nwait = [0]
            for op in per.get(engname, []):
                w = {}
                for d in op.deps:
                    k, v = ops[d].sig
                    if w.get(k, 0) < v:
                        w[k] = v
                for k, v in w.items():
                    if waited.get(k, 0) < v:
                        eng.wait_ge(sems[k], v)
                        waited[k] = v
                        nwait[0] += 1
                ins = op.fn(eng)
                if op.is_dma:
                    if not isinstance(ins, (list, tuple)):
                        ins = [ins]
                    assert len(ins) == op.ninc
                    for i_ in ins:
                        i_.then_inc(sems[op.sig[0]], 16)
                elif op.signal:
                    ins.then_inc(sems[op.sig[0]], 1)
            self.icount[engname] = self.icount.get(engname, 0) + len(per.get(engname, [])) + nwait[0]
            if engname == "sp":
                for key, c in final_dma.items():
                    k = ("dma", key)
                    if waited.get(k, 0) < c:
                        eng.wait_ge(sems[k], c)

        with nc.Block() as block:
            @block.tensor
            def _(e):
                run("pe", e)

            @block.scalar
            def _(e):
                run("act", e)

            @block.vector
            def _(e):
                run("dve", e)

            @block.gpsimd
            def _(e):
                run("pool", e)

            @block.sync
            def _(e):
                run("sp", e)
        print("sched: %d ops, %d sems, max dma sem %d, engine counts %s epochs %s" % (
            len(ops), len(sems), max(dmacnt.values()) if dmacnt else 0, cnt, epoch))
        print("instr per engine (ops+waits):", self.icount)


class Builder:
    def __init__(self, S, NSEQ, layers):
        self.S = S
        self.NSEQ = NSEQ
        self.layers = layers
        self.nc = bass.Bass("TRN2", target_bir_lowering=False)
        self.T = Sched(self.nc)
        self.din = {}
        self.uid = 0
        self.scr_idx = {}
        self.scr_pool = {}
        self.scr_n = 0

    def dram_in(self, name, shape):
        t = self.nc.dram_tensor(name, list(shape), F32, kind="ExternalInput")
        self.din[name] = t
        return t

    def dram_scratch(self, name, shape, dt):
        key = (tuple(shape), str(dt))
        idx = self.scr_idx.get(key, 0)
        self.scr_idx[key] = idx + 1
        pool = self.scr_pool.setdefault(key, [])
        if idx >= len(pool):
            self.scr_n += 1
            pool.append(self.nc.dram_tensor("scr%d" % self.scr_n, list(shape), dt, kind="Internal"))
        return pool[idx]

    def sb(self, stack, name, shape, dt):
        self.uid += 1
        return stack.enter_context(self.nc.sbuf_tensor("%s_%d" % (name, self.uid), list(shape), dt))

    def ps(self, stack, name, shape, dt=F32):
        self.uid += 1
        full = 512 if dt == F32 else 1024
        assert len(shape) == 2 and shape[1] <= full
        t = stack.enter_context(self.nc.psum_tensor("%s_%d" % (name, self.uid), [128, full], dt))
        return t[0:shape[0], 0:shape[1]]

    def dma(self, key, out, in_, reads=(), writes=()):
        return self.T.add("sp", lambda e: e.dma_start(out=out, in_=in_), reads=reads, writes=writes, key=key)

    def act(self, out, in_, func, reads, writes, bias=None, scale=None, accum_out=None):
        kw = {}
        if bias is not None:
            kw["bias"] = bias
        if scale is not None:
            kw["scale"] = scale
        if accum_out is not None:
            kw["accum_out"] = accum_out
        return self.T.add("act", lambda e: e.activation(out=out, in_=in_, func=func, **kw), reads=reads, writes=writes)

    def mm(self, out, lhsT, rhs, start, stop, reads, writes):
        return self.T.add("pe", lambda e: e.matmul(out, lhsT, rhs, start=start, stop=stop), reads=reads, writes=writes)

    def tr(self, out, in_, ident, reads, writes):
        return self.T.add("pe", lambda e: e.transpose(out, in_, ident), reads=reads, writes=writes)

    def v(self, eng, name, reads, writes, *a, **kw):
        return self.T.add(eng, lambda e: getattr(e, name)(*a, **kw), reads=reads, writes=writes)

    def rsqrt(self, out, in_, eps, reads, writes, tmp=None, r_tmp=None):
        if tmp is None:
            tmp, r_tmp = out, writes[0]
        self.act(tmp, in_, AF.Sqrt, reads, [r_tmp], bias=self.eps_ap(eps, tmp.shape[0]))
        return self.v("dve", "reciprocal", [r_tmp], writes, out=out, in_=tmp)

    def eps_ap(self, eps, p):
        return self.epsc[eps][0:p, 0:1]

    def barrier(self):
        nc = self.nc
        sc = self.bar_sc
        fns = {
            "pe": lambda e: e.matmul(self.bar_ps[0:1, 0:1], sc[0:1, 0:1], sc[0:1, 0:1], start=True, stop=True),
            "act": lambda e: e.copy(out=sc[0:1, 2:3], in_=sc[0:1, 1:2]),
            "dve": lambda e: e.memset(sc[0:1, 3:4], 0.0),
            "pool": lambda e: e.memset(sc[0:1, 4:5], 0.0),
            "sp": lambda e: e.nop(),
        }
        self.T.barrier(["pe", "act", "dve", "pool", "sp"], fns)

    def setup_consts(self, stack):
        nc, T = self.nc, self.T
        self.bar_sc = self.sb(stack, "barsc", [1, 8], BF16)
        self.bar_ps = self.ps(stack, "barps", [1, 8], F32)
        self.r_const = Reg("const")
        c_d = self.dram_in("c_ident", [128, 128])
        self.ident_f = self.sb(stack, "identf", [128, 128], F32)
        self.ident_b = self.sb(stack, "identb", [128, 128], BF16)
        self.ones_b = self.sb(stack, "onesb", [128, 128], BF16)
        self.ones_f = self.sb(stack, "onesf", [128, 128], F32)
        self.blk64 = self.sb(stack, "blk64", [128, 128], BF16)
        self.dma("c0", self.ident_f[:], c_d.ap(), writes=[self.r_const])
        self.epsc = {}
        for eps in (1e-6, 64e-5, 1.0):
            t_ = self.sb(stack, "eps", [128, 1], F32)
            T.add("dve", lambda e, t_=t_, eps=eps: e.memset(t_[:], eps), writes=[self.r_const])
            self.epsc[eps] = t_
        T.add("dve", lambda e: e.memset(self.bar_sc[:], 0.0), writes=[self.r_const])
        T.add("dve", lambda e: e.tensor_copy(out=self.ident_b[:], in_=self.ident_f[:]), reads=[self.r_const], writes=[self.r_const])
        T.add("dve", lambda e: e.memset(self.ones_b[:], 1.0), writes=[self.r_const])
        T.add("dve", lambda e: e.memset(self.ones_f[:], 1.0), writes=[self.r_const])
        T.add("dve", lambda e: e.memset(self.blk64[:], 0.0), writes=[self.r_const])
        T.add("dve", lambda e: e.memset(self.blk64[0:64, 0:64], 1.0 / 64), reads=[self.r_const], writes=[self.r_const])
        T.add("dve", lambda e: e.memset(self.blk64[64:128, 64:128], 1.0 / 64), reads=[self.r_const], writes=[self.r_const])

    def phase_hT(self, x_ap, g_ap, hT, hT_regs, norm=True):
        nc, T, S = self.nc, self.T, self.S
        nt = S // 128
        with ExitStack() as st:
            NB = 2
            xdt = F32 if norm else BF16
            xt = [self.sb(st, "xt", [128, D], xdt) for _ in range(NB)]
            r_xt = [Reg() for _ in range(NB)]
            if norm:
                gt = self.sb(st, "gt", [128, D], F32)
                r_gt = Reg()
                self.dma("gt", gt[:], g_ap.partition_broadcast(128), writes=[r_gt])
                junk = self.sb(st, "junk", [128, D], BF16)
                r_junk = Reg()
                ss = [self.sb(st, "ss", [128, 1], F32) for _ in range(NB)]
                rs = [self.sb(st, "rs", [128, 1], F32) for _ in range(NB)]
                r_ss = [Reg() for _ in range(NB)]
                r_rs = [Reg() for _ in range(NB)]
                hn = [self.sb(st, "hn", [128, D], BF16) for _ in range(NB)]
                r_hn = [Reg() for _ in range(NB)]
            tp = [self.ps(st, "tp", [128, 512], BF16) for _ in range(4)]
            r_tp = [Reg() for _ in range(4)]
            for i in range(nt):
                b = i % NB
                self.dma("xt%d" % b, xt[b][:], x_ap[i * 128:(i + 1) * 128, :], writes=[r_xt[b]])
                if norm:
                    self.act(junk[:], xt[b][:], AF.Square, [r_xt[b]], [r_junk, r_ss[b]],
                             scale=float(D ** -0.5), accum_out=ss[b][:])
                    self.rsqrt(rs[b][:], ss[b][:], 1e-6, [r_ss[b]], [r_rs[b]])
                    self.v("dve", "scalar_tensor_tensor", [r_xt[b], r_rs[b], r_gt], [r_hn[b]],
                           out=hn[b][:], in0=xt[b][:], scalar=rs[b][:, 0:1], in1=gt[:],
                           op0=ALU.mult, op1=ALU.mult)
                    src, r_src = hn[b], r_hn[b]
                else:
                    src, r_src = xt[b], r_xt[b]
                for g in range(4):
                    pb = (i * 4 + g) % 4
                    for j in range(4):
                        k = g * 4 + j
                        self.tr(tp[pb][:, j * 128:(j + 1) * 128], src[:, k * 128:(k + 1) * 128],
                                self.ident_b[:], [r_src, self.r_const], [r_tp[pb]])
                    o = hT[:, g * 4:(g + 1) * 4, i * 128:(i + 1) * 128]
                    s_ = tp[pb][:].rearrange("p (k t) -> p k t", k=4)
                    if g % 2 == 0:
                        self.v("dve", "tensor_copy", [r_tp[pb]], [hT_regs[i][g]], out=o, in_=s_)
                    else:
                        self.T.add("act", lambda e, o=o, s_=s_: e.copy(out=o, in_=s_), reads=[r_tp[pb]], writes=[hT_regs[i][g]])
        self.barrier()

    def phase_proj(self, hT, hT_regs, jobs):
        nc, T, S = self.nc, self.T, self.S
        nt = S // 128
        with ExitStack() as st:
            stage = [self.sb(st, "wst", [128, 4, 512], F32) for _ in range(2)]
            r_stage = [Reg() for _ in range(2)]
            wbf = [self.sb(st, "wbf", [128, KC, 512], BF16) for _ in range(2)]
            r_wbf = [[Reg() for _ in range(4)] for _ in range(2)]
            NPS = 3
            pss = [self.ps(st, "pj", [128, 512], F32) for _ in range(NPS)]
            r_ps = [Reg() for _ in range(NPS)]
            pcount = 0
            scount = 0
            for ji, (W_ap, n, orient, cb) in enumerate(jobs):
                wb = ji % 2
                Wv = W_ap.rearrange("(k p) n -> p k n", p=128)
                for qt in range(4):
                    sbi = scount % 2
                    scount += 1
                    self.dma("wst%d" % sbi, stage[sbi][:, :, :n], Wv[:, qt * 4:(qt + 1) * 4, :],
                             writes=[r_stage[sbi]])
                    self.v("pool", "tensor_copy", [r_stage[sbi]], [r_wbf[wb][qt]],
                           out=wbf[wb][:, qt * 4:(qt + 1) * 4, :n], in_=stage[sbi][:, :, :n])
                if orient == "tok":
                    for i in range(nt):
                        p = pcount % NPS
                        pcount += 1
                        for k in range(KC):
                            self.mm(pss[p][:, :n], hT[:, k, i * 128:(i + 1) * 128], wbf[wb][:, k, :n],
                                    k == 0, k == KC - 1,
                                    [hT_regs[i][k // 4], r_wbf[wb][k // 4]], [r_ps[p]])
                        cb(pss[p][:, :n], r_ps[p], i)
                else:
                    nfb = (n + 127) // 128
                    for fb in range(nfb):
                        fn = min(128, n - fb * 128)
                        for tg in range(S // 512):
                            p = pcount % NPS
                            pcount += 1
                            for k in range(KC):
                                rr = [hT_regs[tg * 4 + q][k // 4] for q in range(4)]
                                self.mm(pss[p][:fn, :], wbf[wb][:, k, fb * 128:fb * 128 + fn],
                                        hT[:, k, tg * 512:(tg + 1) * 512], k == 0, k == KC - 1,
                                        rr + [r_wbf[wb][k // 4]], [r_ps[p]])
                            cb(pss[p][:fn, :], r_ps[p], fb, tg, fn)
        self.barrier()

    def qknorm_cb(self, st, gain_col, dst_T, key):
        sq = [self.sb(st, "sq", [128, 512], BF16) for _ in range(2)]
        r_sq = [Reg(), Reg()]
        ssp = [self.ps(st, "ssp", [128, 512], F32) for _ in range(2)]
        r_ssp = [Reg(), Reg()]
        rstd = [self.sb(st, "rstd", [128, 512], F32) for _ in range(2)]
        r_rstd = [Reg(), Reg()]
        qn = [self.sb(st, "qn", [128, 512], BF16) for _ in range(2)]
        r_qn = [Reg(), Reg()]
        cnt = [0]

        def cb(ps, r_ps, fb, tg, fn):
            b = cnt[0] % 2
            cnt[0] += 1
            self.act(sq[b][:fn, :], ps, AF.Square, [r_ps], [r_sq[b]])
            self.mm(ssp[b][:fn, :], self.blk64[:fn, :fn], sq[b][:fn, :], True, True, [r_sq[b], self.r_const], [r_ssp[b]])
            self.rsqrt(rstd[b][:fn, :], ssp[b][:fn, :], 1e-6, [r_ssp[b]], [r_rstd[b]])
            self.v("dve", "scalar_tensor_tensor", [r_ps, r_rstd[b], self.r_const], [r_qn[b]],
                   out=qn[b][:fn, :], in0=ps, scalar=gain_col[:fn, 0:1], in1=rstd[b][:fn, :],
                   op0=ALU.mult, op1=ALU.mult)
            self.dma("%s%d" % (key, b), dst_T[fb * 128:fb * 128 + fn, tg * 512:(tg + 1) * 512], qn[b][:fn, :],
                     reads=[r_qn[b]])
        return cb

    def store_cb(self, st, dst, key, func=None, feat=False, col0=0, scale=None):
        ev = [self.sb(st, "ev", [128, 512], BF16) for _ in range(2)]
        r_ev = [Reg(), Reg()]
        cnt = [0]

        def go(ps, r_ps, b, dst_ap, pn, fn_):
            o = ev[b][:pn, :fn_]
            if func is None and scale is None and cnt[0] % 2 == 0:
                self.v("dve", "tensor_copy", [r_ps], [r_ev[b]], out=o, in_=ps)
            else:
                self.act(o, ps, func if func is not None else AF.Copy, [r_ps], [r_ev[b]], scale=scale)
            self.dma("%s%d" % (key, b), dst_ap, o, reads=[r_ev[b]])

        def cb_tok(ps, r_ps, i):
            b = cnt[0] % 2
            cnt[0] += 1
            n = ps.shape[1]
            go(ps, r_ps, b, dst[i * 128:(i + 1) * 128, col0:col0 + n], 128, n)

        def cb_feat(ps, r_ps, fb, tg, fn):
            b = cnt[0] % 2
            cnt[0] += 1
            go(ps, r_ps, b, dst[col0 + fb * 128:col0 + fb * 128 + fn, tg * 512:(tg + 1) * 512], fn, 512)
        return cb_feat if feat else cb_tok

    def load_col(self, st, vec_ap, n, reps, mul=None):
        t = self.sb(st, "col", [n * reps, 1], F32)
        r = Reg()
        for j in range(reps):
            self.dma("col", t[j * n:(j + 1) * n, :], vec_ap.rearrange("o n -> n o"), writes=[r])
        if mul is not None:
            self.v("dve", "tensor_scalar", [r], [r], out=t[:], in0=t[:], scalar1=float(mul), scalar2=None, op0=ALU.mult)
        return t, r

    def out_proj(self, st, zT, z_regs, w_out_ap, x_ap, xn_ap):
        xr = [self.sb(st, "xr", [128, 512], F32) for _ in range(3)]
        r_xr = [Reg() for _ in range(3)]
        cnt = [0]
        jobs = []
        for cg in range(4):
            def cb(ps, r_ps, i, cg=cg):
                b = cnt[0] % 3
                cnt[0] += 1
                self.dma("xr%d" % b, xr[b][:], x_ap[i * 128:(i + 1) * 128, cg * 512:(cg + 1) * 512], writes=[r_xr[b]])
                self.v("dve", "tensor_tensor", [r_ps, r_xr[b]], [r_xr[b]], out=xr[b][:], in0=ps, in1=xr[b][:], op=ALU.add)
                self.dma("xr%d" % b, xn_ap[i * 128:(i + 1) * 128, cg * 512:(cg + 1) * 512], xr[b][:], reads=[r_xr[b]])
            jobs.append((w_out_ap[:, cg * 512:(cg + 1) * 512], 512, "tok", cb))
        self.phase_proj(zT, z_regs, jobs)

    def load_zT(self, zT, z_regs, zT_d):
        S = self.S
        rg = [Reg() for _ in range(4)]
        for k in range(KC):
            self.dma("zt%d" % (k % 4), zT[:, k, :], zT_d[k * 128:(k + 1) * 128, :], writes=[rg[k // 4]])
        for i in range(S // 128):
            for g in range(4):
                z_regs[i][g] = rg[g]

    def new_regs(self):
        return [[Reg() for _ in range(4)] for _ in range(self.S // 128)]

    def layer_swa(self, x_ap, xn_ap, P, seq):
        nc, T, S = self.nc, self.T, self.S
        nb = S // 128
        W = P["swa_w_in"].ap()[0]
        qT_d = self.dram_scratch("swa_qT%d" % seq, [2048, S], BF16).ap()
        kT_d = self.dram_scratch("swa_kT%d" % seq, [256, S], BF16).ap()
        v_d = self.dram_scratch("swa_v%d" % seq, [S, 256], BF16).ap()
        sgT_d = self.dram_scratch("swa_sgT%d" % seq, [2048, S], BF16).ap()
        zT_d = self.dram_scratch("swa_zT%d" % seq, [2048, S], BF16).ap()
        with ExitStack() as st:
            hT = self.sb(st, "hT", [128, KC, S], BF16)
            regs = self.new_regs()
            self.phase_hT(x_ap, P["swa_norm"].ap(), hT, regs)
            with ExitStack() as st2:
                qg, _ = self.load_col(st2, P["swa_q_gain"].ap(), 64, 2, mul=0.125)
                kg, _ = self.load_col(st2, P["swa_k_gain"].ap(), 64, 2)
                cbq = self.qknorm_cb(st2, qg, qT_d, "qs")
                cbk = self.qknorm_cb(st2, kg, kT_d, "ks")
                cbv = self.store_cb(st2, v_d, "vs")
                cbg = self.store_cb(st2, sgT_d, "gs", func=AF.Silu, feat=True)
                jobs = []
                for c in range(4):
                    def cq(ps, r, fb, tg, fn, c=c):
                        cbq(ps, r, c * 4 + fb, tg, fn)
                    jobs.append((W[:, c * 512:(c + 1) * 512], 512, "feat", cq))
                jobs.append((W[:, 2048:2304], 256, "feat", cbk))
                jobs.append((W[:, 2304:2560], 256, "tok", cbv))
                for c in range(4):
                    def cg_(ps, r, fb, tg, fn, c=c):
                        cbg(ps, r, c * 4 + fb, tg, fn)
                    jobs.append((W[:, 2560 + c * 512:2560 + (c + 1) * 512], 512, "feat", cg_))
                self.phase_proj(hT, regs, jobs)
        with ExitStack() as st2:
            BT = self.swa_bias(st2, P)
            r_BT = self.r_BT
            es = self.sb(st2, "es", [64, 32], F32)
            r_es = Reg()
            self.dma("es", es[:], P["swa_sinks"].ap().partition_broadcast(64), writes=[r_es])
            self.act(es[:], es[:], AF.Exp, [r_es], [r_es])
            vt = self.sb(st2, "vt", [128, nb, 256], BF16)
            r_vt = Reg()
            self.dma("vt", vt[:], v_d.rearrange("(b p) c -> p b c", p=128), writes=[r_vt])
            q = [self.sb(st2, "q", [64, 4, S], BF16) for _ in range(2)]
            sg = [self.sb(st2, "sg", [64, 4, S], BF16) for _ in range(2)]
            kt = [self.sb(st2, "kt", [64, S], BF16) for _ in range(2)]
            ob = [self.sb(st2, "ob", [64, 4, S], BF16) for _ in range(2)]
            r_q = [Reg(), Reg()]
            r_sg = [Reg(), Reg()]
            r_kt = [Reg(), Reg()]
            r_ob = [Reg(), Reg()]
            sps = [self.ps(st2, "sps", [128, 512], F32) for _ in range(2)]
            r_sps = [Reg(), Reg()]
            ops_ = [self.ps(st2, "ops", [64, 512], F32) for _ in range(2)]
            rps = [self.ps(st2, "rps", [64, 512], F32) for _ in range(2)]
            r_ops = [Reg(), Reg()]
            r_rps = [Reg(), Reg()]
            pt = [self.sb(st2, "pt", [128, 512], BF16) for _ in range(3)]
            r_pt = [Reg() for _ in range(3)]
            den = [self.sb(st2, "den", [64, 512], F32) for _ in range(2)]
            r_den = [Reg(), Reg()]
            ot = [self.sb(st2, "ot", [64, 512], F32) for _ in range(2)]
            r_ot = [Reg(), Reg()]
            c_s = 0
            c_p = 0
            c_o = 0
            for g in range(4):
                for hq in range(2):
                    gb = (g * 2 + hq) % 2
                    f0 = g * 512 + hq * 256
                    h0 = g * 8 + hq * 4
                    self.dma("q%d" % gb, q[gb][:], qT_d[f0:f0 + 256, :].rearrange("(h d) t -> d h t", d=64), writes=[r_q[gb]])
                    self.dma("sg%d" % gb, sg[gb][:], sgT_d[f0:f0 + 256, :].rearrange("(h d) t -> d h t", d=64), writes=[r_sg[gb]])
                    self.dma("kt%d" % gb, kt[gb][:], kT_d[g * 64:(g + 1) * 64, :], writes=[r_kt[gb]])
                    for i in range(nb):
                        o_i = c_o % 2
                        c_o += 1
                        kbs = [i - 1, i] if i > 0 else [i]
                        for kk, kb in enumerate(kbs):
                            blk = 0 if kb == i - 1 else 1
                            s_i = c_s % 2
                            c_s += 1
                            p_i = c_p % 3
                            c_p += 1
                            self.mm(sps[s_i][:], kt[gb][:, kb * 128:(kb + 1) * 128],
                                    q[gb][:, :, i * 128:(i + 1) * 128], True, False,
                                    [r_kt[gb], r_q[gb]], [r_sps[s_i]])
                            self.mm(sps[s_i][:], self.ident_b[:], BT[blk][:, h0:h0 + 4, :], False, True,
                                    [r_BT, self.r_const], [r_sps[s_i]])
                            self.act(pt[p_i][:], sps[s_i][:], AF.Exp, [r_sps[s_i]], [r_pt[p_i]])
                            self.mm(ops_[o_i][:], vt[:, kb, g * 64:(g + 1) * 64], pt[p_i][:], kk == 0, kk == len(kbs) - 1,
                                    [r_vt, r_pt[p_i]], [r_ops[o_i]])
                            self.mm(rps[o_i][:], self.ones_b[:, 0:64], pt[p_i][:], kk == 0, kk == len(kbs) - 1,
                                    [self.r_const, r_pt[p_i]], [r_rps[o_i]])
                        self.v("dve", "tensor_tensor", [r_rps[o_i], r_es], [r_den[o_i]],
                               out=den[o_i][:].rearrange("p (h t) -> p h t", h=4),
                               in0=rps[o_i][:].rearrange("p (h t) -> p h t", h=4),
                               in1=es[:, h0:h0 + 4].unsqueeze(2).to_broadcast([64, 4, 128]), op=ALU.add)
                        self.v("dve", "reciprocal", [r_den[o_i]], [r_den[o_i]], out=den[o_i][:], in_=den[o_i][:])
                        self.v("dve", "tensor_tensor", [r_ops[o_i], r_den[o_i]], [r_ot[o_i]],
                               out=ot[o_i][:], in0=ops_[o_i][:], in1=den[o_i][:], op=ALU.mult)
                        self.v("pool", "tensor_tensor", [r_ot[o_i], r_sg[gb]], [r_ob[gb]],
                               out=ob[gb][:, :, i * 128:(i + 1) * 128],
                               in0=ot[o_i][:].rearrange("p (h t) -> p h t", h=4),
                               in1=sg[gb][:, :, i * 128:(i + 1) * 128], op=ALU.mult)
                    self.dma("ob%d" % gb, zT_d[f0:f0 + 256, :].rearrange("(h d) t -> d h t", d=64), ob[gb][:], reads=[r_ob[gb]])
        self.barrier()
        with ExitStack() as st2:
            zT = self.sb(st2, "zT", [128, KC, S], BF16)
            z_regs = self.new_regs()
            self.load_zT(zT, z_regs, zT_d)
            self.out_proj(st2, zT, z_regs, P["swa_w_out"].ap()[0], x_ap, xn_ap)

    def swa_bias(self, st, P):
        T = self.T
        oh = self.sb(st, "oh", [32, 128], F32)
        tab = self.sb(st, "tab", [32, 32], F32)
        A = self.sb(st, "antidiag", [128, 384], F32)
        msk = self.sb(st, "msk", [128, 2, 128], F32)
        r0 = Reg()
        self.dma("sb0", oh[:], self.din["c_onehot"].ap(), writes=[r0])
        self.dma("sb1", tab[:], P["rel_bias"].ap(), writes=[r0])
        self.dma("sb2", A[:], self.din["c_antidiag"].ap(), writes=[r0])
        self.dma("sb3", msk[:], self.din["c_swamask"].ap(), writes=[r0])
        f = self.sb(st, "fdh", [128, 32], F32)
        BT = [self.sb(st, "BT", [128, 32, 128], BF16) for _ in range(2)]
        self.r_BT = Reg()
        with ExitStack() as s2:
            fp = self.ps(s2, "fp", [128, 32], F32)
            r_fp = Reg()
            r_f = Reg()
            self.mm(fp[:], oh[:], tab[:], True, True, [r0], [r_fp])
            self.v("dve", "tensor_copy", [r_fp], [r_f], out=f[:], in_=fp[:])
            bp = [self.ps(s2, "bp", [128, 512], F32) for _ in range(2)]
            r_bp = [Reg(), Reg()]
            c = 0
            for blk in range(2):
                for q0 in range(0, 128, 16):
                    b = c % 2
                    c += 1
                    for qq in range(16):
                        qi = q0 + qq
                        off = (255 - qi) if blk == 1 else (127 - qi)
                        self.mm(bp[b][:, qq * 32:(qq + 1) * 32], A[:, off:off + 128], f[:], True, True,
                                [r0, r_f], [r_bp[b]])
                    self.v("dve", "tensor_tensor", [r_bp[b], r0], [self.r_BT],
                           out=BT[blk][:, :, q0:q0 + 16].rearrange("p h q -> p q h"),
                           in0=bp[b][:].rearrange("p (q h) -> p q h", h=32),
                           in1=msk[:, blk, q0:q0 + 16].unsqueeze(2).to_broadcast([128, 16, 32]), op=ALU.add)
        return BT

    def ls_bufs(self, st, pn, fn):
        return (self.sb(st, "ls_x", [pn, fn], F32), self.sb(st, "ls_a", [pn, fn], F32), Reg(), Reg())

    def log_sigmoid(self, bufs, out, ps, r_ps, bias_col, r_out, pn, fn):
        xs, a, r1, r2 = bufs
        self.act(xs[:], ps, AF.Identity, [r_ps, self.r_const], [r1], bias=bias_col)
        self.v("dve", "scalar_tensor_tensor", [r1], [r2], out=a[:], in0=xs[:], scalar=-1.0, in1=xs[:],
               op0=ALU.mult, op1=ALU.min)
        self.act(a[:], a[:], AF.Exp, [r2], [r2])
        self.act(a[:], a[:], AF.Ln, [r2], [r2], bias=self.eps_ap(1.0, pn))
        self.v("dve", "scalar_tensor_tensor", [r1, r2], [r_out], out=out, in0=xs[:], scalar=0.0, in1=a[:],
               op0=ALU.min, op1=ALU.subtract)

    def layer_fox(self, x_ap, xn_ap, P, seq):
        nc, T, S = self.nc, self.T, self.S
        nb = S // 128
        W = P["fox_w_in"].ap()[0]
        qT_d = self.dram_scratch("fox_qT%d" % seq, [2048, S], BF16).ap()
        kT_d = self.dram_scratch("fox_kT%d" % seq, [2048, S], BF16).ap()
        v_d = self.dram_scratch("fox_v%d" % seq, [S, 2048], BF16).ap()
        sgT_d = self.dram_scratch("fox_sgT%d" % seq, [2048, S], BF16).ap()
        zT_d = self.dram_scratch("fox_zT%d" % seq, [2048, S], BF16).ap()
        Fa_d = self.dram_scratch("fox_Fa%d" % seq, [32, 6, S], BF16).ap()
        with ExitStack() as st:
            logf = self.sb(st, "logf", [32, S], F32)
            r_logf = Reg()
            hT = self.sb(st, "hT", [128, KC, S], BF16)
            regs = self.new_regs()
            self.phase_hT(x_ap, P["fox_norm"].ap(), hT, regs)
            with ExitStack() as st2:
                qg, _ = self.load_col(st2, P["fox_q_gain"].ap(), 64, 2, mul=0.125)
                kg, _ = self.load_col(st2, P["fox_k_gain"].ap(), 64, 2)
                bf, r_bf = self.load_col(st2, P["fox_b_f"].ap(), 32, 1)
                cbq = self.qknorm_cb(st2, qg, qT_d, "qs")
                cbk = self.qknorm_cb(st2, kg, kT_d, "ks")
                cbg = self.store_cb(st2, sgT_d, "gs", func=AF.Silu, feat=True)
                jobs = []
                for c in range(4):
                    def cq(ps, r, fb, tg, fn, c=c):
                        cbq(ps, r, c * 4 + fb, tg, fn)
                    jobs.append((W[:, c * 512:(c + 1) * 512], 512, "feat", cq))
                for c in range(4):
                    def ck(ps, r, fb, tg, fn, c=c):
                        cbk(ps, r, c * 4 + fb, tg, fn)
                    jobs.append((W[:, 2048 + c * 512:2048 + (c + 1) * 512], 512, "feat", ck))
                for c in range(4):
                    jobs.append((W[:, 4096 + c * 512:4096 + (c + 1) * 512], 512, "tok",
                                 self.store_cb(st2, v_d, "vs", col0=c * 512)))
                for c in range(4):
                    def cg_(ps, r, fb, tg, fn, c=c):
                        cbg(ps, r, c * 4 + fb, tg, fn)
                    jobs.append((W[:, 6144 + c * 512:6144 + (c + 1) * 512], 512, "feat", cg_))

                lsb = self.ls_bufs(st2, 32, 512)

                def cf(ps, r, fb, tg, fn):
                    self.log_sigmoid(lsb, logf[:, tg * 512:(tg + 1) * 512], ps, r, bf[:, 0:1], r_logf, 32, 512)
                jobs.append((W[:, 8192:8224], 32, "feat", cf))
                self.phase_proj(hT, regs, jobs)
            with ExitStack() as st2:
                fab = (self.sb(st2, "fo", [32, S], F32), self.sb(st2, "fF", [32, S], F32),
                       self.sb(st2, "ft", [32, S], F32), self.sb(st2, "fa", [32, 6, S], BF16))
                self.fox_faug(fab, logf, r_logf, Fa_d)
            self.barrier()
        with ExitStack() as st2:
            cm_f = self.sb(st2, "cmf", [128, 128], F32)
            cm = self.sb(st2, "cm", [128, 128], BF16)
            r_cm = Reg()
            self.dma("cm", cm_f[:], self.din["c_causal"].ap(), writes=[r_cm])
            self.v("dve", "tensor_copy", [r_cm], [r_cm], out=cm[:], in_=cm_f[:])
            Qa = [self.sb(st2, "Qa", [128, S], BF16) for _ in range(2)]
            Ka = [self.sb(st2, "Ka", [128, S], BF16) for _ in range(2)]
            r_Qa = [Reg(), Reg()]
            r_Ka = [Reg(), Reg()]
            for b in range(2):
                self.v("pool", "memset", [], [r_Qa[b]], Qa[b][:], 0.0)
                self.v("pool", "memset", [], [r_Qa[b]], Qa[b][96:128, :], 1.0)
                self.v("pool", "memset", [], [r_Ka[b]], Ka[b][:], 0.0)
                self.v("pool", "memset", [], [r_Ka[b]], Ka[b][64:96, :], 1.0)
            vt = [self.sb(st2, "vt", [128, nb, 512], BF16) for _ in range(2)]
            r_vt = [Reg(), Reg()]
            sg = [self.sb(st2, "sg", [64, S], BF16) for _ in range(2)]
            ob = [self.sb(st2, "ob", [64, S], BF16) for _ in range(2)]
            r_sg = [Reg(), Reg()]
            r_ob = [Reg(), Reg()]
            sps = [self.ps(st2, "sps", [128, 512], F32) for _ in range(2)]
            r_sps = [Reg(), Reg()]
            ops_ = [self.ps(st2, "ops", [64, 512], F32) for _ in range(2)]
            rps = [self.ps(st2, "rps", [64, 512], F32) for _ in range(2)]
            r_ops = [Reg(), Reg()]
            r_rps = [Reg(), Reg()]
            pt = [self.sb(st2, "pt", [128, 512], BF16) for _ in range(3)]
            r_pt = [Reg() for _ in range(3)]
            den = [self.sb(st2, "den", [64, 512], F32) for _ in range(2)]
            r_den = [Reg(), Reg()]
            ot = [self.sb(st2, "ot", [64, 512], F32) for _ in range(2)]
            r_ot = [Reg(), Reg()]
            c_s = c_p = c_o = 0
            for h in range(32):
                hb = h % 2
                if h % 8 == 0:
                    vb = (h // 8) % 2
                    self.dma("vt%d" % vb, vt[vb][:], v_d[:, (h // 8) * 512:(h // 8 + 1) * 512].rearrange("(b p) c -> p b c", p=128),
                             writes=[r_vt[vb]])
                self.dma("qa%d" % hb, Qa[hb][0:64, :], qT_d[h * 64:(h + 1) * 64, :], writes=[r_Qa[hb]])
                self.dma("qa%d" % hb, Qa[hb][64:67, :], Fa_d[h, 0:3, :], writes=[r_Qa[hb]])
                self.dma("ka%d" % hb, Ka[hb][0:64, :], kT_d[h * 64:(h + 1) * 64, :], writes=[r_Ka[hb]])
                self.dma("ka%d" % hb, Ka[hb][96:99, :], Fa_d[h, 3:6, :], writes=[r_Ka[hb]])
                self.dma("sg%d" % hb, sg[hb][:], sgT_d[h * 64:(h + 1) * 64, :], writes=[r_sg[hb]])
                hc = (h % 8) * 64
                for sblk in range(S // 512):
                    o_i = c_o % 2
                    c_o += 1
                    nj = 4 * sblk + 4
                    for j in range(nj):
                        q0 = max(sblk * 512, j * 128)
                        q1 = (sblk + 1) * 512
                        n = q1 - q0
                        c0 = q0 - sblk * 512
                        s_i = c_s % 2
                        c_s += 1
                        p_i = c_p % 3
                        c_p += 1
                        diag = j >= 4 * sblk
                        self.mm(sps[s_i][:, :n], Ka[hb][:, j * 128:(j + 1) * 128], Qa[hb][:, q0:q1], True, not diag,
                                [r_Ka[hb], r_Qa[hb]], [r_sps[s_i]])
                        if diag:
                            self.mm(sps[s_i][:, 0:128], self.ident_b[:], cm[:], False, True,
                                    [r_cm, self.r_const], [r_sps[s_i]])
                        self.act(pt[p_i][:, :n], sps[s_i][:, :n], AF.Exp, [r_sps[s_i]], [r_pt[p_i]])
                        self.mm(ops_[o_i][:, c0:512], vt[vb][:, j, hc:hc + 64], pt[p_i][:, :n], j == 0, j == nj - 1,
                                [r_vt[vb], r_pt[p_i]], [r_ops[o_i]])
                        self.mm(rps[o_i][:, c0:512], self.ones_b[:, 0:64], pt[p_i][:, :n], j == 0, j == nj - 1,
                                [self.r_const, r_pt[p_i]], [r_rps[o_i]])
                    self.v("dve", "reciprocal", [r_rps[o_i]], [r_den[o_i]], out=den[o_i][:], in_=rps[o_i][:])
                    self.v("dve", "tensor_tensor", [r_ops[o_i], r_den[o_i]], [r_ot[o_i]],
                           out=ot[o_i][:], in0=ops_[o_i][:], in1=den[o_i][:], op=ALU.mult)
                    self.v("pool", "tensor_tensor", [r_ot[o_i], r_sg[hb]], [r_ob[hb]],
                           out=ob[hb][:, sblk * 512:(sblk + 1) * 512], in0=ot[o_i][:],
                           in1=sg[hb][:, sblk * 512:(sblk + 1) * 512], op=ALU.mult)
                self.dma("ob%d" % hb, zT_d[h * 64:(h + 1) * 64, :], ob[hb][:], reads=[r_ob[hb]])
        self.barrier()
        with ExitStack() as st2:
            zT = self.sb(st2, "zT", [128, KC, S], BF16)
            z_regs = self.new_regs()
            self.load_zT(zT, z_regs, zT_d)
            self.out_proj(st2, zT, z_regs, P["fox_w_out"].ap()[0], x_ap, xn_ap)

    def fox_faug(self, bufs, logf, r_logf, Fa_d):
        S = self.S
        ones, F, t32, Fa = bufs
        r = Reg()
        ra = Reg()
        self.v("dve", "memset", [], [r], ones[:], 1.0)
        self.v("dve", "tensor_tensor_scan", [r, r_logf], [r], out=F[:], data0=ones[:], data1=logf[:], initial=0.0,
               op0=ALU.mult, op1=ALU.add)
        for p in range(3):
            self.v("dve", "tensor_copy", [r], [ra], out=Fa[:, p, :], in_=F[:])
            if p < 2:
                self.v("dve", "tensor_copy", [ra], [r], out=t32[:], in_=Fa[:, p, :])
                self.v("dve", "tensor_tensor", [r], [r], out=F[:], in0=F[:], in1=t32[:], op=ALU.subtract)
        self.v("dve", "tensor_scalar", [ra], [ra], out=Fa[:, 3:6, :], in0=Fa[:, 0:3, :], scalar1=-1.0, scalar2=None,
               op0=ALU.mult)
        self.dma("fa", Fa_d, Fa[:], reads=[ra])

    def layer_mlstm(self, x_ap, xn_ap, P, seq):
        nc, T, S = self.nc, self.T, self.S
        nb = S // 128
        NG = nb * 8
        W = P["mlstm_w_in"].ap()[0]
        qT_d = self.dram_scratch("ml_qT%d" % seq, [1024, S], BF16).ap()
        kT_d = self.dram_scratch("ml_kT%d" % seq, [1024, S], BF16).ap()
        k_d = self.dram_scratch("ml_k%d" % seq, [S, 1024], BF16).ap()
        v_d = self.dram_scratch("ml_v%d" % seq, [S, 2048], BF16).ap()
        so_d = self.dram_scratch("ml_so%d" % seq, [S, 2048], BF16).ap()
        sg_d = self.dram_scratch("ml_sg%d" % seq, [S, 2048], BF16).ap()
        z_d = self.dram_scratch("ml_z%d" % seq, [S, 2048], BF16).ap()
        with ExitStack() as st0:
            G = self.sb(st0, "G", [128, nb, 16], F32)
            r_G = Reg()
            with ExitStack() as st:
                hT = self.sb(st, "hT", [128, KC, S], BF16)
                regs = self.new_regs()
                self.phase_hT(x_ap, P["mlstm_norm"].ap(), hT, regs)
                with ExitStack() as st2:
                    jobs = []
                    cbq = self.store_cb(st2, qT_d, "qs", feat=True, scale=float(128 ** -0.5))
                    cbkT = self.store_cb(st2, kT_d, "ks", feat=True)
                    for c in range(2):
                        def cq(ps, r, fb, tg, fn, c=c):
                            cbq(ps, r, c * 4 + fb, tg, fn)
                        jobs.append((W[:, c * 512:(c + 1) * 512], 512, "feat", cq))
                    for c in range(2):
                        def ck(ps, r, fb, tg, fn, c=c):
                            cbkT(ps, r, c * 4 + fb, tg, fn)
                        jobs.append((W[:, 1024 + c * 512:1024 + (c + 1) * 512], 512, "feat", ck))
                        jobs.append((W[:, 1024 + c * 512:1024 + (c + 1) * 512], 512, "tok",
                                     self.store_cb(st2, k_d, "kk", col0=c * 512)))
                    for c in range(4):
                        jobs.append((W[:, 2048 + c * 512:2048 + (c + 1) * 512], 512, "tok",
                                     self.store_cb(st2, v_d, "vs", col0=c * 512)))
                    for c in range(4):
                        jobs.append((W[:, 4096 + c * 512:4096 + (c + 1) * 512], 512, "tok",
                                     self.store_cb(st2, so_d, "so", func=AF.Sigmoid, col0=c * 512)))
                    for c in range(4):
                        jobs.append((W[:, 6144 + c * 512:6144 + (c + 1) * 512], 512, "tok",
                                     self.store_cb(st2, sg_d, "sgs", func=AF.Silu, col0=c * 512)))

                    def cif(ps, r, i):
                        self.v("dve", "tensor_copy", [r], [r_G], out=G[:, i, :], in_=ps)
                    jobs.append((W[:, 8192:8208], 16, "tok", cif))
                    self.phase_proj(hT, regs, jobs)
            with ExitStack() as st2:
                def t_(name, shape, dt=F32):
                    return self.sb(st2, name, shape, dt)
                bi = t_("bi", [128, 8])
                bfv = t_("bfv", [128, 8])
                r_b = Reg()
                self.dma("mlb", bi[:], P["mlstm_b_i"].ap().partition_broadcast(128), writes=[r_b])
                self.dma("mlb", bfv[:], P["mlstm_b_f"].ap().partition_broadcast(128), writes=[r_b])
                triu = t_("triu", [128, 128])
                r_tri = Reg()
                self.dma("mlt", triu[:], self.din["c_triu"].ap(), writes=[r_tri])
                gain = t_("gain", [128, 2048])
                r_gain = Reg()
                self.dma("mlg", gain[:], P["mlstm_h_gain"].ap().rearrange("o h v -> o (h v)").partition_broadcast(128), writes=[r_gain])
                il = t_("il", [128, nb, 8]); fx = t_("fx", [128, nb, 8]); fa = t_("fa", [128, nb, 8]); fl = t_("fl", [128, nb, 8])
                bb = t_("bb", [128, NG]); u = t_("u", [128, NG]); E1 = t_("E1", [128, NG]); thr = t_("thr", [128, NG])
                apB = t_("apB", [128, 2 * NG])
                Ucol = t_("Ucol", [128, 1])
                rows = t_("rows", [1, 6, NG])
                r_g = Reg()
                bc = lambda tl: tl[:].unsqueeze(1).to_broadcast([128, nb, 8])
                self.v("dve", "tensor_tensor", [r_G, r_b], [r_g], out=il[:], in0=G[:, :, 0:8], in1=bc(bi), op=ALU.add)
                self.v("dve", "tensor_tensor", [r_G, r_b], [r_g], out=fx[:], in0=G[:, :, 8:16], in1=bc(bfv), op=ALU.add)
                self.v("dve", "scalar_tensor_tensor", [r_g], [r_g], out=fa[:], in0=fx[:], scalar=-1.0, in1=fx[:], op0=ALU.mult, op1=ALU.min)
                self.act(fa[:], fa[:], AF.Exp, [r_g], [r_g])
                self.act(fa[:], fa[:], AF.Ln, [r_g], [r_g], bias=self.eps_ap(1.0, 128))
                self.v("dve", "scalar_tensor_tensor", [r_g], [r_g], out=fl[:], in0=fx[:], scalar=0.0, in1=fa[:], op0=ALU.min, op1=ALU.subtract)
                flf = fl[:].rearrange("p c h -> p (c h)")
                ilf = il[:].rearrange("p c h -> p (c h)")
                with ExitStack() as st3:
                    pA = self.ps(st3, "pA", [128, 2 * NG], F32)
                    pB = self.ps(st3, "pB", [128, 128], F32)
                    pR = self.ps(st3, "pR", [1, 2 * NG], F32)
                    r_pA, r_pB, r_pR = Reg(), Reg(), Reg()
                    self.mm(pA[:, 0:NG], triu[:], flf, True, True, [r_tri, r_g], [r_pA])
                    self.v("dve", "tensor_copy", [r_pA], [r_g], out=bb[:], in_=pA[:, 0:NG])
                    self.v("dve", "tensor_tensor", [r_g], [r_g], out=u[:], in0=ilf, in1=bb[:], op=ALU.subtract)
                    self.mm(pR[:, 0:NG], self.ones_f[:, 0:1], flf, True, True, [self.r_const, r_g], [r_pR])
                    self.v("dve", "tensor_copy", [r_pR], [r_g], out=rows[:, 1, :], in_=pR[:, 0:NG])
                    self.tr(pB[:NG, :], u[:], self.ident_f[:], [r_g, self.r_const], [r_pB])
                    self.v("dve", "reduce_max", [r_pB], [r_g], out=Ucol[:NG, :], in_=pB[:NG, :], axis=AX.X)
                    self.mm(pR[:, NG:2 * NG], Ucol[:NG, :], self.ident_f[:NG, :NG], True, True, [r_g, self.r_const], [r_pR])
                    self.v("dve", "tensor_copy", [r_pR], [r_g], out=rows[:, 0, :], in_=pR[:, NG:2 * NG])
                    rv = lambda j: rows[:, j, :].rearrange("p (c h) -> p c h", h=8)
                    for c in range(nb):
                        cs = slice(c * 8, (c + 1) * 8)
                        if c == 0:
                            self.v("dve", "tensor_scalar", [r_g], [r_g], out=rows[:, 2, cs], in0=rows[:, 0, cs], scalar1=0.0,
                                   scalar2=None, op0=ALU.max)
                        else:
                            self.v("dve", "tensor_tensor", [r_g], [r_g], out=rows[:, 2, cs], in0=rows[:, 0, cs],
                                   in1=rows[:, 2, (c - 1) * 8:c * 8], op=ALU.max)
                        self.v("dve", "tensor_tensor", [r_g], [r_g], out=rows[:, 2, cs], in0=rows[:, 2, cs],
                               in1=rows[:, 1, cs], op=ALU.add)
                    self.v("dve", "tensor_tensor", [r_g], [r_g], out=rows[:, 3, :], in0=rows[:, 2, :], in1=rows[:, 1, :], op=ALU.subtract)
                    self.v("dve", "memset", [r_g], [r_g], rows[:, 5, :], 0.0)
                    if nb > 1:
                        self.v("dve", "tensor_copy", [r_g], [r_g], out=rows[:, 5, 8:NG], in_=rows[:, 2, 0:NG - 8])
                    self.v("dve", "tensor_tensor", [r_g], [r_g], out=rows[:, 4, :], in0=rows[:, 5, :], in1=rows[:, 3, :], op=ALU.subtract)
                    self.act(rows[:, 4, :], rows[:, 4, :], AF.Exp, [r_g], [r_g])
                    self.mm(pA[:], self.ones_f[0:1, :], rows[:, 3:5, :].rearrange("p a n -> p (a n)"), True, True,
                            [self.r_const, r_g], [r_pA])
                    self.v("dve", "tensor_copy", [r_pA], [r_g], out=apB[:], in_=pA[:])
                    self.v("dve", "tensor_tensor", [r_g], [r_g], out=E1[:], in0=u[:], in1=apB[:, 0:NG], op=ALU.subtract)
                    self.act(E1[:], E1[:], AF.Exp, [r_g], [r_g])
                    self.v("dve", "tensor_tensor", [r_g], [r_g], out=thr[:], in0=bb[:], in1=apB[:, 0:NG], op=ALU.add)
                    self.act(thr[:], thr[:], AF.Exp, [r_g], [r_g], scale=-1.0)
                lam = apB[:, NG:2 * NG]
                qT = self.sb(st2, "qT", [128, 8, S], BF16)
                kT = self.sb(st2, "kT", [128, 8, S], BF16)
                ktok = self.sb(st2, "ktok", [128, nb, 1024], BF16)
                r_ld = Reg()
                for h in range(8):
                    self.dma("mq%d" % (h % 2), qT[:, h, :], qT_d[h * 128:(h + 1) * 128, :], writes=[r_ld])
                    self.dma("mk%d" % (h % 2), kT[:, h, :], kT_d[h * 128:(h + 1) * 128, :], writes=[r_ld])
                self.dma("mkt", ktok[:], k_d.rearrange("(b p) c -> p b c", p=128), writes=[r_ld])
                vp = [self.sb(st2, "vp", [128, 8, 257], BF16) for _ in range(2)]
                so = [self.sb(st2, "so", [128, 2048], BF16) for _ in range(2)]
                sgt = [self.sb(st2, "sgt", [128, 2048], BF16) for _ in range(2)]
                zt = [self.sb(st2, "zt", [128, 2048], BF16) for _ in range(2)]
                r_vp = [Reg(), Reg()]; r_so = [Reg(), Reg()]; r_sgt = [Reg(), Reg()]; r_zt = [Reg(), Reg()]
                for b in range(2):
                    self.v("pool", "memset", [], [r_vp[b]], vp[b][:], 1.0)
                CT = [self.sb(st2, "CT", [128, 257], F32) for _ in range(8)]
                CTb = [self.sb(st2, "CTb", [128, 257], BF16) for _ in range(8)]
                r_CT = [Reg() for _ in range(8)]
                r_CTb = [Reg() for _ in range(8)]
                for h in range(8):
                    self.v("pool", "memset", [], [r_CT[h]], CT[h][:], 0.0)
                sps = [self.ps(st2, "sps", [128, 128], F32) for _ in range(2)]
                ndp = [self.ps(st2, "ndp", [128, 257], F32) for _ in range(2)]
                dcp = [self.ps(st2, "dcp", [128, 257], F32) for _ in range(2)]
                r_sps = [Reg(), Reg()]; r_ndp = [Reg(), Reg()]; r_dcp = [Reg(), Reg()]
                swt = [self.sb(st2, "swt", [128, 128], BF16) for _ in range(2)]
                Vs = [self.sb(st2, "Vs", [128, 257], BF16) for _ in range(2)]
                r_swt = [Reg(), Reg()]; r_Vs = [Reg(), Reg()]
                sm = [self.sb(st2, "sm", [128, 8], F32) for _ in range(2)]
                r_sm = [Reg(), Reg()]
                hh = [self.sb(st2, "hh", [128, 256], F32) for _ in range(2)]
                hj = self.sb(st2, "hj", [128, 256], BF16)
                r_hh = [Reg(), Reg()]
                r_hj = Reg()
                cc = 0
                for c in range(nb):
                    cb_ = c % 2
                    tok = slice(c * 128, (c + 1) * 128)
                    self.dma("mv%d" % cb_, vp[cb_][:, :, 0:256], v_d[tok, :].rearrange("p (h v) -> p h v", h=8), writes=[r_vp[cb_]])
                    self.dma("mso%d" % cb_, so[cb_][:], so_d[tok, :], writes=[r_so[cb_]])
                    self.dma("msg%d" % cb_, sgt[cb_][:], sg_d[tok, :], writes=[r_sgt[cb_]])
                    self.v("pool", "tensor_tensor", [r_so[cb_], r_sgt[cb_]], [r_so[cb_]], out=so[cb_][:], in0=so[cb_][:], in1=sgt[cb_][:], op=ALU.mult)
                    for h in range(8):
                        b = cc % 2
                        cc += 1
                        col = c * 8 + h
                        self.mm(sps[b][:], kT[:, h, tok], qT[:, h, tok], True, True, [r_ld], [r_sps[b]])
                        self.v("dve", "tensor_tensor", [r_sps[b], r_tri], [r_swt[b]], out=swt[b][:], in0=sps[b][:], in1=triu[:], op=ALU.mult)
                        self.act(Vs[b][:], vp[cb_][:, h, :], AF.Copy, [r_vp[cb_], r_g], [r_Vs[b]], scale=E1[:, col:col + 1])
                        self.v("dve", "tensor_scalar", [r_CT[h], r_g], [r_CT[h]], out=CT[h][:], in0=CT[h][:], scalar1=lam[:, col:col + 1],
                               scalar2=0.0, op0=ALU.mult, op1=ALU.add)
                        self.v("pool", "tensor_copy", [r_CT[h]], [r_CTb[h]], out=CTb[h][:], in_=CT[h][:])
                        self.mm(ndp[b][:], qT[:, h, tok], CTb[h][:], True, False, [r_ld, r_CTb[h]], [r_ndp[b]])
                        self.mm(ndp[b][:], swt[b][:], Vs[b][:], False, True, [r_swt[b], r_Vs[b]], [r_ndp[b]])
                        self.mm(dcp[b][:], ktok[:, c, h * 128:(h + 1) * 128], Vs[b][:], True, True, [r_ld, r_Vs[b]], [r_dcp[b]])
                        self.v("dve", "tensor_tensor", [r_CT[h], r_dcp[b]], [r_CT[h]], out=CT[h][:], in0=CT[h][:], in1=dcp[b][:], op=ALU.add)
                        self.v("dve", "tensor_copy", [r_ndp[b]], [r_sm[b]], out=sm[b][:, 4:5], in_=ndp[b][:, 256:257])
                        self.v("dve", "scalar_tensor_tensor", [r_sm[b]], [r_sm[b]], out=sm[b][:, 0:1], in0=sm[b][:, 4:5], scalar=-1.0,
                               in1=sm[b][:, 4:5], op0=ALU.mult, op1=ALU.max)
                        self.v("dve", "tensor_tensor", [r_sm[b], r_g], [r_sm[b]], out=sm[b][:, 0:1], in0=sm[b][:, 0:1], in1=thr[:, col:col + 1], op=ALU.max)
                        self.v("dve", "reciprocal", [r_sm[b]], [r_sm[b]], out=sm[b][:, 1:2], in_=sm[b][:, 0:1])
                        self.act(hh[b][:], ndp[b][:, 0:256], AF.Copy, [r_ndp[b], r_sm[b]], [r_hh[b]], scale=sm[b][:, 1:2])
                        self.act(hj[:], hh[b][:], AF.Square, [r_hh[b]], [r_hj, r_sm[b]], scale=1.0 / 16, accum_out=sm[b][:, 2:3])
                        self.rsqrt(sm[b][:, 3:4], sm[b][:, 2:3], 1e-6, [r_sm[b]], [r_sm[b]])
                        self.v("dve", "scalar_tensor_tensor", [r_hh[b], r_sm[b], r_gain], [r_hh[b]], out=hh[b][:], in0=hh[b][:],
                               scalar=sm[b][:, 3:4], in1=gain[:, h * 256:(h + 1) * 256], op0=ALU.mult, op1=ALU.mult)
                        self.v("pool", "tensor_tensor", [r_hh[b], r_so[cb_]], [r_zt[cb_]], out=zt[cb_][:, h * 256:(h + 1) * 256], in0=hh[b][:],
                               in1=so[cb_][:, h * 256:(h + 1) * 256], op=ALU.mult)
                    self.dma("mz%d" % cb_, z_d[tok, :], zt[cb_][:], reads=[r_zt[cb_]])
        self.barrier()
        with ExitStack() as st2:
            zT = self.sb(st2, "zT", [128, KC, S], BF16)
            z_regs = self.new_regs()
            self.phase_hT(z_d, None, zT, z_regs, norm=False)
            self.out_proj(st2, zT, z_regs, P["mlstm_w_out"].ap()[0], x_ap, xn_ap)

    def load_cols(self, st, dst, c0, vec_ap, nrows, key):
        n = nrows * 16
        with ExitStack() as s2:
            A = self.sb(s2, "lcA", [n, 128], F32)
            pp = self.ps(s2, "lcp", [128, n], F32)
            r, rp = Reg(), Reg()
            self.dma(key, A[:], vec_ap.rearrange("r (k p) -> (r k) p", p=128), writes=[r])
            self.tr(pp[:], A[:], self.ident_f[:n, :n], [r, self.r_const], [rp])
            self.v("dve", "tensor_copy", [rp], [self.r_pc], out=dst[:, c0:c0 + n], in_=pp[:])
        self.barrier()

    def layer_rwkv(self, x_ap, xn_ap, P, seq):
        nc, T, S = self.nc, self.T, self.S
        nb = S // 128
        C = 64
        nch = S // C
        c0e = float(np.exp(-0.5))
        W4 = P["rwkv_w_in"].ap()[0]
        dsc = lambda n, shp, dt=BF16: self.dram_scratch("rw_%s%d" % (n, seq), shp, dt).ap()
        rT_d, kT_d = dsc("rT", [2048, S]), dsc("kT", [2048, S])
        v_d, sg_d, z_d = dsc("v", [S, 2048]), dsc("sg", [S, 2048]), dsc("z", [S, 2048])
        at_d, bt_d, kt_d, rt_d = dsc("at", [2048, S]), dsc("bt", [2048, S]), dsc("kt", [2048, S]), dsc("rt", [2048, S])
        y_d = dsc("y", [S, 2048], F32)
        ge_d = dsc("ge", [2048, nch], F32)
        with ExitStack() as st0:
            pc = self.sb(st0, "pc", [128, 11 * 16], F32)
            omm = self.sb(st0, "omm", [128, 96], F32)
            self.r_pc = Reg()
            self.load_cols(st0, pc, 0, P["rwkv_mix"].ap()[0], 6, "lc")
            for j, nm in enumerate(["rwkv_w0", "rwkv_a0", "rwkv_k_k", "rwkv_k_a"]):
                self.load_cols(st0, pc, 96 + j * 16, P[nm].ap(), 1, "lc")
            self.load_cols(st0, pc, 160, P["rwkv_r_k"].ap().rearrange("o h n -> o (h n)"), 1, "lc")
            self.v("dve", "tensor_scalar", [self.r_pc], [self.r_pc], out=omm[:], in0=pc[:, 0:96], scalar1=-1.0, scalar2=1.0,
                   op0=ALU.mult, op1=ALU.add)
            r_pc = self.r_pc
            PC = lambda r, k: pc[:, r * 16 + k:r * 16 + k + 1]
            t1T = self.sb(st0, "t1T", [96, S], BF16)
            a1T = self.sb(st0, "a1T", [96, S], BF16)
            r_t1, r_a1 = Reg(), Reg()
            bonus = self.sb(st0, "bonus", [128, nb, 32], F32)
            r_bonus = Reg()
            with ExitStack() as st:
                hT = self.sb(st, "hT", [128, KC, S], BF16)
                regs = self.new_regs()
                self.phase_hT(x_ap, P["rwkv_norm"].ap(), hT, regs)
                xmT = self.sb(st, "xmT", [128, KC, S], BF16)
                for cv in range(6):
                    rg = [Reg() for _ in range(4)]
                    xm_regs = [[rg[g] for g in range(4)] for _ in range(nb)]
                    for k in range(KC):
                        eng = "dve" if k % 2 == 0 else "pool"
                        hr = [regs[i][k // 4] for i in range(nb)]
                        self.v("pool", "tensor_scalar", hr + [r_pc], [rg[k // 4]], out=xmT[:, k, :], in0=hT[:, k, :],
                               scalar1=omm[:, cv * 16 + k:cv * 16 + k + 1], scalar2=0.0, op0=ALU.mult, op1=ALU.add)
                        self.v("dve", "scalar_tensor_tensor", hr + [r_pc, rg[k // 4]], [rg[k // 4]], out=xmT[:, k, 1:S],
                               in0=hT[:, k, 0:S - 1], scalar=PC(cv, k), in1=xmT[:, k, 1:S], op0=ALU.mult, op1=ALU.add)
                    with ExitStack() as st2:
                        jobs = []
                        if cv < 4:
                            if cv == 0:
                                cbx = self.store_cb(st2, rT_d, "qs", feat=True)
                            elif cv == 1:
                                cbx = self.store_cb(st2, kT_d, "ks", feat=True)
                            for c in range(4):
                                Wc = W4[cv][:, c * 512:(c + 1) * 512]
                                if cv < 2:
                                    def cf_(ps, r, fb, tg, fn, c=c, cbx=cbx):
                                        cbx(ps, r, c * 4 + fb, tg, fn)
                                    jobs.append((Wc, 512, "feat", cf_))
                                elif cv == 2:
                                    jobs.append((Wc, 512, "tok", self.store_cb(st2, v_d, "vs", col0=c * 512)))
                                else:
                                    jobs.append((Wc, 512, "tok", self.store_cb(st2, sg_d, "sgs", func=AF.Silu, col0=c * 512)))
                        elif cv == 4:
                            def cw(ps, r, fb, tg, fn):
                                self.act(t1T[:, tg * 512:(tg + 1) * 512], ps, AF.Tanh, [r], [r_t1])
                            jobs.append((P["rwkv_w_lora1"].ap()[0], 96, "feat", cw))
                        else:
                            def ca(ps, r, fb, tg, fn):
                                self.act(a1T[:, tg * 512:(tg + 1) * 512], ps, AF.Copy, [r], [r_a1])
                            jobs.append((P["rwkv_a_lora1"].ap()[0], 96, "feat", ca))
                        self.phase_proj(xmT, xm_regs, jobs)
            with ExitStack() as st:
                f32t = lambda n: self.sb(st, n, [128, S], F32)
                b16t = lambda n: self.sb(st, n, [128, S], BF16)
                l2s = self.sb(st, "l2s", [96, 2048], F32)
                wl2 = self.sb(st, "wl2", [96, 2048], BF16)
                al2 = self.sb(st, "al2", [96, 2048], BF16)
                r_l2 = Reg()
                r_l2s = Reg()
                self.dma("l2", l2s[:], P["rwkv_w_lora2"].ap()[0], writes=[r_l2s])
                self.v("dve", "tensor_copy", [r_l2s], [r_l2], out=wl2[:], in_=l2s[:])
                self.dma("l2", l2s[:], P["rwkv_a_lora2"].ap()[0], reads=[r_l2], writes=[r_l2s])
                self.v("dve", "tensor_copy", [r_l2s], [r_l2], out=al2[:], in_=l2s[:])
                rmask = f32t("rmask")
                hind = self.sb(st, "hind", [128, 2], BF16)
                blk1 = self.sb(st, "blk1", [128, 128], BF16)
                tiny = self.sb(st, "tiny", [128, 1], F32)
                r_k0 = Reg()
                self.v("pool", "memset", [], [r_k0], rmask[:], 1.0)
                self.v("pool", "memset", [r_k0], [r_k0], rmask[:].rearrange("p (c t) -> p c t", t=C)[:, :, 0:1], 0.0)
                self.v("pool", "memset", [], [r_k0], hind[:], 0.0)
                self.v("pool", "memset", [r_k0], [r_k0], hind[0:64, 0:1], 1.0)
                self.v("pool", "memset", [r_k0], [r_k0], hind[64:128, 1:2], 1.0)
                self.v("pool", "memset", [], [r_k0], blk1[:], 0.0)
                self.v("pool", "memset", [r_k0], [r_k0], blk1[0:64, 0:64], 1.0)
                self.v("pool", "memset", [r_k0], [r_k0], blk1[64:128, 64:128], 1.0)
                self.v("pool", "memset", [], [r_k0], tiny[:], 1e-24)
                sigw, aa, cums, Ep, Em, Epv = f32t("sigw"), f32t("aa"), f32t("cums"), f32t("Ep"), f32t("Em"), f32t("Epv")
                kkr, rn, kp, tt = f32t("kkr"), f32t("rn"), f32t("kp"), f32t("tt")
                rj, kj, sqb, prod = b16t("rj"), b16t("kj"), b16t("sqb"), b16t("prod")
                o_r, o_k, o_b, o_a = b16t("o_r"), b16t("o_k"), b16t("o_b"), b16t("o_a")
                gend = self.sb(st, "gend", [128, nch], F32)
                R = {n: Reg() for n in ["sigw", "aa", "cums", "Ep", "Em", "Epv", "kkr", "rn", "kp", "tt", "rj", "kj", "sqb", "prod",
                                        "o_r", "o_k", "o_b", "o_a", "gend"]}
                pd = [self.ps(st, "pd", [128, 512], F32) for _ in range(3)]
                r_pd = [Reg() for _ in range(3)]
                pbn = self.ps(st, "pbn", [128, nb * 2], F32)
                r_pbn = Reg()
                pc_ = 0
                ntg = S // 512
                for j in range(KC):
                    fs = slice(j * 128, (j + 1) * 128)
                    self.dma("drj", rj[:], rT_d[fs, :], writes=[R["rj"]])
                    self.dma("dkj", kj[:], kT_d[fs, :], writes=[R["kj"]])
                    for tg in range(ntg):
                        ts_ = slice(tg * 512, (tg + 1) * 512)
                        p = pc_ % 3
                        pc_ += 1
                        self.mm(pd[p][:], wl2[:, fs], t1T[:, ts_], True, True, [r_l2, r_t1], [r_pd[p]])
                        self.act(sigw[:, ts_], pd[p][:], AF.Sigmoid, [r_pd[p], r_pc], [R["sigw"]], bias=PC(6, j))
                        p = pc_ % 3
                        pc_ += 1
                        self.mm(pd[p][:], al2[:, fs], a1T[:, ts_], True, True, [r_l2, r_a1], [r_pd[p]])
                        self.act(aa[:, ts_], pd[p][:], AF.Sigmoid, [r_pd[p], r_pc], [R["aa"]], bias=PC(7, j))
                    self.v("dve", "tensor_tensor_scan", [r_k0, R["sigw"]], [R["cums"]], out=cums[:], data0=rmask[:], data1=sigw[:],
                           initial=0.0, op0=ALU.mult, op1=ALU.add)
                    self.act(Ep[:], cums[:], AF.Exp, [R["cums"]], [R["Ep"]], scale=-c0e)
                    self.act(Em[:], cums[:], AF.Exp, [R["cums"]], [R["Em"]], scale=c0e)
                    self.v("pool", "tensor_tensor", [R["cums"], R["sigw"]], [R["tt"]], out=tt[:], in0=cums[:], in1=sigw[:], op=ALU.subtract)
                    self.act(Epv[:], tt[:], AF.Exp, [R["tt"]], [R["Epv"]], scale=-c0e)
                    self.v("dve", "tensor_scalar", [R["kj"], r_pc], [R["kkr"]], out=kkr[:], in0=kj[:], scalar1=PC(8, j), scalar2=0.0,
                           op0=ALU.mult, op1=ALU.add)
                    self.act(sqb[:], kkr[:], AF.Square, [R["kkr"]], [R["sqb"]])
                    for tg in range(ntg):
                        ts_ = slice(tg * 512, (tg + 1) * 512)
                        p = pc_ % 3
                        pc_ += 1
                        self.mm(pd[p][:], blk1[:], sqb[:, ts_], True, True, [r_k0, R["sqb"]], [r_pd[p]])
                        self.act(rn[:, ts_], pd[p][:], AF.Sqrt, [r_pd[p], r_k0], [R["rn"]], bias=tiny[:, 0:1])
                    self.v("dve", "reciprocal", [R["rn"]], [R["rn"]], out=rn[:], in_=rn[:])
                    self.v("dve", "tensor_tensor", [R["kkr"], R["rn"]], [R["kkr"]], out=kkr[:], in0=kkr[:], in1=rn[:], op=ALU.mult)
                    self.v("pool", "tensor_scalar", [R["aa"], r_pc, R["Epv"]], [R["tt"]], out=tt[:], in0=aa[:], scalar1=-1.0, scalar2=PC(9, j),
                           op0=ALU.add, op1=ALU.mult)
                    self.v("dve", "scalar_tensor_tensor", [R["tt"], R["kj"]], [R["kp"]], out=kp[:], in0=tt[:], scalar=1.0, in1=kj[:],
                           op0=ALU.add, op1=ALU.mult)
                    self.v("dve", "tensor_tensor", [R["rj"], R["Ep"]], [R["o_r"]], out=o_r[:], in0=rj[:], in1=Ep[:], op=ALU.mult)
                    self.v("pool", "tensor_tensor", [R["kp"], R["Em"]], [R["o_k"]], out=o_k[:], in0=kp[:], in1=Em[:], op=ALU.mult)
                    self.v("dve", "tensor_tensor", [R["kkr"], R["aa"]], [R["tt"]], out=tt[:], in0=kkr[:], in1=aa[:], op=ALU.mult)
                    self.v("pool", "tensor_tensor", [R["tt"], R["Em"]], [R["o_b"]], out=o_b[:], in0=tt[:], in1=Em[:], op=ALU.mult)
                    self.v("dve", "scalar_tensor_tensor", [R["kkr"], R["Epv"]], [R["o_a"]], out=o_a[:], in0=kkr[:], scalar=-1.0, in1=Epv[:],
                           op0=ALU.mult, op1=ALU.mult)
                    self.v("pool", "tensor_copy", [R["Ep"]], [R["gend"]], out=gend[:],
                           in_=Ep[:].rearrange("p (c t) -> p c t", t=C)[:, :, C - 1])
                    self.v("dve", "scalar_tensor_tensor", [R["kp"], R["rj"], r_pc], [R["prod"]], out=prod[:], in0=kp[:], scalar=PC(10, j), in1=rj[:],
                           op0=ALU.mult, op1=ALU.mult)
                    for i in range(nb):
                        self.mm(pbn[:, i * 2:(i + 1) * 2], prod[:, i * 128:(i + 1) * 128], hind[:], True, True, [R["prod"], r_k0], [r_pbn])
                    self.v("dve", "tensor_copy", [r_pbn], [r_bonus], out=bonus[:, :, j * 2:(j + 1) * 2],
                           in_=pbn[:].rearrange("p (i h) -> p i h", h=2))
                    self.dma("dor", rt_d[fs, :], o_r[:], reads=[R["o_r"]])
                    self.dma("dok", kt_d[fs, :], o_k[:], reads=[R["o_k"]])
                    self.dma("dob", bt_d[fs, :], o_b[:], reads=[R["o_b"]])
                    self.dma("doa", at_d[fs, :], o_a[:], reads=[R["o_a"]])
                    self.dma("dge", ge_d[fs, :], gend[:], reads=[R["gend"]])
            self.barrier()
            with ExitStack() as st:
                m2f = self.sb(st, "m2f", [64, 2, 64], F32)
                mLf = self.sb(st, "mLf", [64, 64], F32)
                r_m = Reg()
                self.dma("rm0", m2f[:, 0, :], self.din["c_triu_strict"].ap()[0:64, 0:64], writes=[r_m])
                self.dma("rm0", m2f[:, 1, :], self.din["c_triu"].ap()[0:64, 0:64], writes=[r_m])
                self.dma("rm0", mLf[:], self.din["c_tril_strict"].ap()[0:64, 0:64], writes=[r_m])
                U = nch
                NG = U // 8
                HB = 2
                mk = lambda n, shp, dt: [self.sb(st, n, shp, dt) for _ in range(HB)]
                AR, BK, BKt = mk("AR", [64, U, 2, 64], BF16), mk("BK", [64, U, 2, 64], BF16), mk("BKt", [64, U, 2, 64], BF16)
                Pm, Qm, Wm = mk("Pm", [64, U, 64], F32), mk("Qm", [64, U, 64], F32), mk("Wm", [64, U, 64], F32)
                Mx = mk("Mx", [64, U, 3, 64], BF16)
                vt = mk("vt", [64, U, 64], BF16)
                Yb = mk("Yb", [64, U, 64], F32)
                gc = mk("gc", [64, U], F32)
                ST = mk("ST", [64, 64], F32)
                STb = mk("STb", [64, 64], BF16)
                Xs = mk("Xs", [64, 64], F32)
                Us = mk("Us", [64, 64], BF16)
                rr = lambda: [Reg() for _ in range(HB)]
                r_AR, r_BK, r_BKt, r_Mx, r_vt, r_Yb, r_gc, r_ST, r_STb, r_Xs, r_Us = (rr() for _ in range(11))
                r_P = [[Reg() for _ in range(NG)] for _ in range(HB)]
                r_Q = [[Reg() for _ in range(NG)] for _ in range(HB)]
                r_W = [[Reg() for _ in range(NG)] for _ in range(HB)]
                pA = [self.ps(st, "pA", [64, 512], F32) for _ in range(2)]
                pB = [self.ps(st, "pB", [64, 512], F32) for _ in range(2)]
                pT = self.ps(st, "pT", [64, 512], BF16)
                r_pA, r_pB, r_pT = [Reg(), Reg()], [Reg(), Reg()], Reg()
                sq = [self.ps(st, "sq", [64, 512], F32) for _ in range(HB)]
                r_sq = [[Reg() for _ in range(8)] for _ in range(HB)]
                ca = cbk = 0
                for hp in range(16):
                    for hh in range(HB):
                        h = hp * 2 + hh
                        fs = slice(h * 64, (h + 1) * 64)
                        cv_ = lambda ap: ap.rearrange("p (c t) -> p c t", t=C)
                        self.dma("ra%d" % hh, AR[hh][:, :, 0, :], cv_(at_d[fs, :]), writes=[r_AR[hh]])
                        self.dma("ra%d" % hh, AR[hh][:, :, 1, :], cv_(rt_d[fs, :]), writes=[r_AR[hh]])
                        self.dma("rb%d" % hh, BK[hh][:, :, 0, :], cv_(bt_d[fs, :]), writes=[r_BK[hh]])
                        self.dma("rb%d" % hh, BK[hh][:, :, 1, :], cv_(kt_d[fs, :]), writes=[r_BK[hh]])
                        self.dma("rv%d" % hh, vt[hh][:], v_d[:, fs].rearrange("(c t) f -> t c f", t=C), writes=[r_vt[hh]])
                        self.dma("rg%d" % hh, gc[hh][:], ge_d[fs, :], writes=[r_gc[hh]])
                        self.v("pool", "memset", [], [r_ST[hh]], ST[hh][:], 0.0)
                        self.v("pool", "memset", [], [r_STb[hh]], STb[hh][:], 0.0)
                        for c4 in range(0, U, 4):
                            for cc in range(4):
                                for j in range(2):
                                    sl = (cc * 2 + j) * 64
                                    self.tr(pT[:, sl:sl + 64], BK[hh][:, c4 + cc, j, :], self.ident_b[0:64, 0:64],
                                            [r_BK[hh], self.r_const], [r_pT])
                            self.v("dve", "tensor_copy", [r_pT], [r_BKt[hh]], out=BKt[hh][:, c4:c4 + 4, :, :],
                                   in_=pT[:].rearrange("p (c j t) -> p c j t", c=4, j=2))
                        for c4 in range(0, U, 4):
                            a = ca % 2
                            ca += 1
                            for cc in range(4):
                                c = c4 + cc
                                self.mm(pA[a][:, cc * 128:(cc + 1) * 128], BK[hh][:, c, 0, :], AR[hh][:, c, :, :], True, True,
                                        [r_BK[hh], r_AR[hh]], [r_pA[a]])
                            g8 = c4 // 8
                            pv = pA[a][:].rearrange("p (c j t) -> p c j t", c=4, j=2)
                            self.v("dve", "tensor_tensor", [r_pA[a], r_m], [r_P[hh][g8]], out=Pm[hh][:, c4:c4 + 4, :], in0=pv[:, :, 0, :],
                                   in1=m2f[:, 0, :].unsqueeze(1).to_broadcast([64, 4, 64]), op=ALU.mult)
                            self.v("dve", "tensor_tensor", [r_pA[a], r_m], [r_Mx[hh]], out=Mx[hh][:, c4:c4 + 4, 0, :], in0=pv[:, :, 1, :],
                                   in1=m2f[:, 1, :].unsqueeze(1).to_broadcast([64, 4, 64]), op=ALU.mult)
                            a = ca % 2
                            ca += 1
                            for cc in range(4):
                                c = c4 + cc
                                self.mm(pA[a][:, cc * 128:(cc + 1) * 128], BK[hh][:, c, 1, :], AR[hh][:, c, :, :], True, True,
                                        [r_BK[hh], r_AR[hh]], [r_pA[a]])
                            pv = pA[a][:].rearrange("p (c j t) -> p c j t", c=4, j=2)
                            self.v("dve", "tensor_tensor", [r_pA[a], r_m], [r_Mx[hh]], out=Mx[hh][:, c4:c4 + 4, 1:3, :], in0=pv,
                                   in1=m2f[:].unsqueeze(1).to_broadcast([64, 4, 2, 64]), op=ALU.mult)
                        for g8 in range(NG):
                            b_ = cbk % 2
                            cbk += 1
                            for cc in range(8):
                                c = g8 * 8 + cc
                                self.mm(pB[b_][:, cc * 64:(cc + 1) * 64], AR[hh][:, c, 0, :], BK[hh][:, c, 0, :], True, True,
                                        [r_BK[hh], r_AR[hh]], [r_pB[b_]])
                            self.v("dve", "tensor_tensor", [r_pB[b_], r_m], [r_Q[hh][g8]], out=Qm[hh][:, g8 * 8:(g8 + 1) * 8, :],
                                   in0=pB[b_][:].rearrange("p (c t) -> p c t", c=8),
                                   in1=mLf[:].unsqueeze(1).to_broadcast([64, 8, 64]), op=ALU.mult)
                            self.v("pool", "tensor_tensor", [r_P[hh][g8], self.r_const], [r_W[hh][g8]], out=Wm[hh][:, g8 * 8:(g8 + 1) * 8, :],
                                   in0=Pm[hh][:, g8 * 8:(g8 + 1) * 8, :],
                                   in1=self.ident_f[0:64, 0:64].unsqueeze(1).to_broadcast([64, 8, 64]), op=ALU.add)
                        for step in range(5):
                            for g8 in range(NG):
                                us = slice(g8 * 8, (g8 + 1) * 8)
                                b1 = cbk % 2
                                cbk += 1
                                b2 = cbk % 2
                                cbk += 1
                                for cc in range(8):
                                    u = g8 * 8 + cc
                                    self.mm(pB[b1][:, cc * 64:(cc + 1) * 64], Qm[hh][:, u, :], Pm[hh][:, u, :], True, True,
                                            [r_Q[hh][g8], r_P[hh][g8]], [r_pB[b1]])
                                for cc in range(8):
                                    u = g8 * 8 + cc
                                    self.mm(pB[b2][:, cc * 64:(cc + 1) * 64], Pm[hh][:, u, :], Qm[hh][:, u, :], True, True,
                                            [r_Q[hh][g8], r_P[hh][g8]], [r_pB[b2]])
                                self.v("dve", "tensor_copy", [r_pB[b1]], [r_P[hh][g8]], out=Pm[hh][:, us, :],
                                       in_=pB[b1][:].rearrange("p (c t) -> p c t", c=8))
                                self.T.add("act", lambda e, o=Qm[hh][:, us, :], i_=pB[b2][:].rearrange("p (c t) -> p c t", c=8): e.copy(out=o, in_=i_),
                                           reads=[r_pB[b2]], writes=[r_Q[hh][g8]])
                                a = ca % 2
                                ca += 1
                                for cc in range(8):
                                    u = g8 * 8 + cc
                                    self.mm(pA[a][:, cc * 64:(cc + 1) * 64], Qm[hh][:, u, :], Wm[hh][:, u, :], True, True,
                                            [r_Q[hh][g8], r_W[hh][g8]], [r_pA[a]])
                                self.v("dve", "tensor_tensor", [r_pA[a], r_W[hh][g8]], [r_W[hh][g8]], out=Wm[hh][:, us, :], in0=Wm[hh][:, us, :],
                                       in1=pA[a][:].rearrange("p (c t) -> p c t", c=8), op=ALU.add)
                    for c in range(U):
                        for hh in range(HB):
                            g8 = c // 8
                            o = (c % 2) * 256
                            xs_, us_, ys_, ss_ = (sq[hh][:, o + i * 64:o + (i + 1) * 64] for i in range(4))
                            rx = ru = ry = rs = r_sq[hh][0]
                            vm = vt[hh][:, c, :]
                            self.mm(xs_, AR[hh][:, c, 0, :], STb[hh][:], True, False, [r_AR[hh], r_STb[hh]], [rx])
                            self.mm(xs_, Mx[hh][:, c, 1, :], vm, False, True, [r_Mx[hh], r_vt[hh]], [rx])
                            self.T.add("act", lambda e, o_=Xs[hh][:], i_=xs_: e.copy(out=o_, in_=i_), reads=[rx], writes=[r_Xs[hh], rx])
                            self.mm(us_, Wm[hh][:, c, :], Xs[hh][:], True, True, [r_W[hh][g8], r_Xs[hh]], [ru])
                            self.v("dve", "tensor_copy", [ru], [r_Us[hh], ru], out=Us[hh][:], in_=us_)
                            self.mm(ys_, AR[hh][:, c, 1, :], STb[hh][:], True, False, [r_AR[hh], r_STb[hh]], [ry])
                            self.mm(ys_, Mx[hh][:, c, 0, :], Us[hh][:], False, False, [r_Mx[hh], r_Us[hh]], [ry])
                            self.mm(ys_, Mx[hh][:, c, 2, :], vm, False, True, [r_Mx[hh], r_vt[hh]], [ry])
                            self.T.add("act", lambda e, o_=Yb[hh][:, c, :], i_=ys_: e.copy(out=o_, in_=i_), reads=[ry], writes=[r_Yb[hh], ry])
                            self.mm(ss_, BKt[hh][:, c, 0, :], Us[hh][:], True, False, [r_BKt[hh], r_Us[hh]], [rs])
                            self.mm(ss_, BKt[hh][:, c, 1, :], vm, False, True, [r_BKt[hh], r_vt[hh]], [rs])
                            self.v("dve", "tensor_tensor", [rs, r_ST[hh]], [r_ST[hh], rs], out=ST[hh][:], in0=ST[hh][:], in1=ss_, op=ALU.add)
                            self.v("dve", "tensor_scalar", [r_ST[hh], r_gc[hh]], [r_ST[hh]], out=ST[hh][:], in0=ST[hh][:],
                                   scalar1=gc[hh][:, c:c + 1], scalar2=0.0, op0=ALU.mult, op1=ALU.add)
                            self.v("pool", "tensor_copy", [r_ST[hh]], [r_STb[hh]], out=STb[hh][:], in_=ST[hh][:])
                    for hh in range(HB):
                        h = hp * 2 + hh
                        self.dma("ry%d" % hh, y_d[:, h * 64:(h + 1) * 64].rearrange("(c t) f -> t c f", t=C), Yb[hh][:], reads=[r_Yb[hh]])
            self.barrier()
            with ExitStack() as st:
                gw = self.sb(st, "gw", [128, 2048], F32)
                gb = self.sb(st, "gb", [128, 2048], F32)
                r_gp = Reg()
                self.dma("pg0", gw[:], P["rwkv_gn_w"].ap().partition_broadcast(128), writes=[r_gp])
                self.dma("pg1", gb[:], P["rwkv_gn_b"].ap().partition_broadcast(128), writes=[r_gp])
                NBF = 2
                yt = [self.sb(st, "yt", [128, 2048], F32) for _ in range(NBF)]
                y2 = [self.sb(st, "y2", [128, 2048], F32) for _ in range(NBF)]
                vv = [self.sb(st, "vv", [128, 2048], BF16) for _ in range(NBF)]
                sgt = [self.sb(st, "sgt", [128, 2048], BF16) for _ in range(NBF)]
                zo = [self.sb(st, "zo", [128, 2048], BF16) for _ in range(NBF)]
                sm = [self.sb(st, "sm", [128, 3, 32], F32) for _ in range(NBF)]
                r_yt, r_y2, r_vv, r_sgt, r_zo, r_sm = ([Reg() for _ in range(NBF)] for _ in range(6))
                h3 = lambda ap: ap.rearrange("p (h n) -> p h n", n=64)
                bc = lambda ap: ap.unsqueeze(2).to_broadcast([128, 32, 64])
                for i in range(nb):
                    b = i % NBF
                    tok = slice(i * 128, (i + 1) * 128)
                    self.dma("py%d" % b, yt[b][:], y_d[tok, :], writes=[r_yt[b]])
                    self.dma("pv%d" % b, vv[b][:], v_d[tok, :], writes=[r_vv[b]])
                    self.dma("ps%d" % b, sgt[b][:], sg_d[tok, :], writes=[r_sgt[b]])
                    self.v("dve", "reduce_sum", [r_yt[b]], [r_sm[b]], out=sm[b][:, 0, :], in_=h3(yt[b][:]), axis=AX.X)
                    self.v("dve", "tensor_scalar", [r_sm[b]], [r_sm[b]], out=sm[b][:, 0, :], in0=sm[b][:, 0, :], scalar1=1.0 / 64, scalar2=0.0,
                           op0=ALU.mult, op1=ALU.add)
                    self.v("dve", "tensor_tensor", [r_yt[b], r_sm[b]], [r_yt[b]], out=h3(yt[b][:]), in0=h3(yt[b][:]), in1=bc(sm[b][:, 0, :]),
                           op=ALU.subtract)
                    self.act(y2[b][:], yt[b][:], AF.Square, [r_yt[b]], [r_y2[b]], scale=0.125)
                    self.v("dve", "reduce_sum", [r_y2[b]], [r_sm[b]], out=sm[b][:, 1, :], in_=h3(y2[b][:]), axis=AX.X)
                    self.rsqrt(sm[b][:, 2, :], sm[b][:, 1, :], 64e-5, [r_sm[b]], [r_sm[b]])
                    self.v("dve", "tensor_tensor", [r_yt[b], r_sm[b]], [r_yt[b]], out=h3(yt[b][:]), in0=h3(yt[b][:]), in1=bc(sm[b][:, 2, :]),
                           op=ALU.mult)
                    self.v("pool", "tensor_tensor", [r_yt[b], r_gp], [r_yt[b]], out=yt[b][:], in0=yt[b][:], in1=gw[:], op=ALU.mult)
                    self.v("pool", "tensor_tensor", [r_yt[b], r_gp], [r_yt[b]], out=yt[b][:], in0=yt[b][:], in1=gb[:], op=ALU.add)
                    self.v("dve", "tensor_tensor", [r_vv[b], r_bonus, r_y2[b]], [r_y2[b]], out=h3(y2[b][:]), in0=h3(vv[b][:]), in1=bc(bonus[:, i, :]),
                           op=ALU.mult)
                    self.v("pool", "tensor_tensor", [r_yt[b], r_y2[b]], [r_yt[b]], out=yt[b][:], in0=yt[b][:], in1=y2[b][:], op=ALU.add)
                    self.v("dve", "tensor_tensor", [r_yt[b], r_sgt[b]], [r_zo[b]], out=zo[b][:], in0=yt[b][:], in1=sgt[b][:], op=ALU.mult)
                    self.dma("pz%d" % b, z_d[tok, :], zo[b][:], reads=[r_zo[b]])
        self.barrier()
        with ExitStack() as st2:
            zT = self.sb(st2, "zT", [128, KC, S], BF16)
            z_regs = self.new_regs()
            self.phase_hT(z_d, None, zT, z_regs, norm=False)
            self.out_proj(st2, zT, z_regs, P["rwkv_w_out"].ap()[0], x_ap, xn_ap)


PARAM_SHAPES = {
    "rel_bias": (32, 32), "swa_norm": (1, 2048), "swa_w_in": (1, 2048, 4608), "swa_q_gain": (1, 64),
    "swa_k_gain": (1, 64), "swa_sinks": (1, 32), "swa_w_out": (1, 2048, 2048),
    "rwkv_norm": (1, 2048), "rwkv_mix": (1, 6, 2048), "rwkv_w_in": (1, 4, 2048, 2048), "rwkv_w0": (1, 2048),
    "rwkv_w_lora1": (1, 2048, 96), "rwkv_w_lora2": (1, 96, 2048), "rwkv_a0": (1, 2048),
    "rwkv_a_lora1": (1, 2048, 96), "rwkv_a_lora2": (1, 96, 2048), "rwkv_k_k": (1, 2048), "rwkv_k_a": (1, 2048),
    "rwkv_r_k": (1, 32, 64), "rwkv_gn_w": (1, 2048), "rwkv_gn_b": (1, 2048), "rwkv_w_out": (1, 2048, 2048),
    "mlstm_norm": (1, 2048), "mlstm_w_in": (1, 2048, 8208), "mlstm_b_i": (1, 8), "mlstm_b_f": (1, 8),
    "mlstm_h_gain": (1, 8, 256), "mlstm_w_out": (1, 2048, 2048),
    "fox_norm": (1, 2048), "fox_w_in": (1, 2048, 8224), "fox_b_f": (1, 32), "fox_q_gain": (1, 64),
    "fox_k_gain": (1, 64), "fox_w_out": (1, 2048, 2048),
}
LAYER_PARAMS = {
    0: ["rel_bias", "swa_norm", "swa_w_in", "swa_q_gain", "swa_k_gain", "swa_sinks", "swa_w_out"],
    1: [k for k in PARAM_SHAPES if k.startswith("rwkv_")],
    2: [k for k in PARAM_SHAPES if k.startswith("mlstm_")],
    3: [k for k in PARAM_SHAPES if k.startswith("fox_")],
}


def t5_bucket_np(dist):
    max_exact = 16
    d_f = np.maximum(dist, 1).astype(np.float32)
    large = max_exact + (np.log(d_f / np.float32(max_exact)) / np.float32(np.log(128 / max_exact))
                         * np.float32(32 - max_exact)).astype(np.int32)
    large = np.minimum(large, 31)
    return np.where(dist < max_exact, dist, large)


def make_consts():
    c = {}
    c["c_ident"] = np.eye(128, dtype=np.float32)
    d = np.arange(128)
    b = t5_bucket_np(d)
    oh = np.zeros((32, 128), np.float32)
    oh[b, d] = 1.0
    c["c_onehot"] = oh
    A = np.zeros((128, 384), np.float32)
    for dd in range(128):
        A[dd, 255 - dd] = 1.0
    c["c_antidiag"] = A
    k = np.arange(128)[:, None]
    q = np.arange(128)[None, :]
    m = np.zeros((128, 2, 128), np.float32)
    m[:, 0, :] = np.where(k <= q, NEG, 0.0)
    m[:, 1, :] = np.where(k > q, NEG, 0.0)
    c["c_swamask"] = m
    c["c_causal"] = np.where(k > q, NEG, 0.0).astype(np.float32)
    c["c_triu"] = (k <= q).astype(np.float32)
    c["c_triu_strict"] = (k < q).astype(np.float32)
    c["c_tril_strict"] = (k > q).astype(np.float32)
    return c


CONST_SHAPES = {k: v.shape for k, v in make_consts().items()}


def build(S, NSEQ, layers):
    B = Builder(S, NSEQ, layers)
    nc = B.nc
    x_d = B.dram_in("x", [NSEQ * S, D])
    need = set()
    for L in layers:
        need |= set(LAYER_PARAMS[L])
    P = {name: B.dram_in(name, PARAM_SHAPES[name]) for name in PARAM_SHAPES if name in need}
    for name, shp in CONST_SHAPES.items():
        if name != "c_ident":
            B.dram_in(name, shp)
    y_d = nc.dram_tensor("y", [NSEQ * S, D], F32, kind="ExternalOutput")
    xs = [nc.dram_tensor("xs%d" % i, [NSEQ * S, D], F32, kind="Internal") for i in range(2)]
    fns = {0: B.layer_swa, 1: getattr(B, "layer_rwkv", None), 2: getattr(B, "layer_mlstm", None),
           3: getattr(B, "layer_fox", None)}
    with ExitStack() as st:
        B.setup_consts(st)
        for seq in range(NSEQ):
            cur = x_d.ap()[seq * S:(seq + 1) * S, :]
            for li, L in enumerate(layers):
                if li == len(layers) - 1:
                    nxt = y_d.ap()[seq * S:(seq + 1) * S, :]
                else:
                    nxt = xs[li % 2].ap()[seq * S:(seq + 1) * S, :]
                B.scr_idx = {}
                fns[L](cur, nxt, P, seq)
                cur = nxt
        B.T.emit()
    return nc, sorted(need)


def _launch(nc, need, xs, inputs, consts):
    in_maps = []
    for c in range(len(xs)):
        m = {"x": np.ascontiguousarray(xs[c], dtype=np.float32)}
        for name in need:
            m[name] = np.ascontiguousarray(inputs[name], dtype=np.float32)
        m.update(consts)
        in_maps.append(m)
    res = run_bass_kernel_spmd(nc, in_maps, core_ids=list(range(len(xs))))
    return [r["y"] for r in res.results]


def kernel(**inputs):
    S, NSEQ, NCORES = 2048, 2, 8
    consts = make_consts()
    x = np.ascontiguousarray(inputs["x"], dtype=np.float32).reshape(NCORES, NSEQ * S, D)
    nc, need = build(S, NSEQ, [0, 1, 2, 3])
    ys = _launch(nc, need, [x[c] for c in range(NCORES)], inputs, consts)
    y = np.stack(ys, axis=0)
    return y.reshape(16, S, D).astype(np.float32)
```

```python
import numpy as np
from contextlib import ExitStack
import concourse.bass as bass
import concourse.mybir as mybir
from concourse.bass_utils import run_bass_kernel_spmd

F32 = mybir.dt.float32
BF16 = mybir.dt.bfloat16
AF = mybir.ActivationFunctionType
ALU = mybir.AluOpType
AX = mybir.AxisListType

D = 2048
KC = 16
NEG = -30000.0


class Reg:
    __slots__ = ("name", "w", "r")

    def __init__(self, name=""):
        self.name = name
        self.w = None
        self.r = []


class Op:
    __slots__ = ("eng", "fn", "deps", "idx", "signal", "sig", "is_dma", "key", "ninc")


class Sched:
    def __init__(self, nc):
        self.nc = nc
        self.ops = []
        self.keyregs = {}
        self.keymap = {}

    def add(self, eng, fn, reads=(), writes=(), key=None, ninc=1):
        op = Op()
        op.eng = eng
        op.fn = fn
        op.idx = len(self.ops)
        op.is_dma = key is not None
        op.key = key
        op.ninc = ninc
        op.signal = False
        op.sig = None
        writes = list(writes)
        if key is not None:
            key = self.keymap.setdefault(key, "k%d" % (len(self.keymap) % 64))
            op.key = key
            kr = self.keyregs.get(key)
            if kr is None:
                kr = self.keyregs[key] = Reg(key)
            writes.append(kr)
        raw = set()
        other = set()
        for r in reads:
            if r.w is not None:
                raw.add(r.w)
        for w in writes:
            if w.w is not None:
                other.add(w.w)
            for x in w.r:
                other.add(x)
        for r in reads:
            r.r.append(op.idx)
        for w in writes:
            w.w = op.idx
            w.r = []
        deps = set()
        ops = self.ops
        for d in raw | other:
            if d == op.idx:
                continue
            o = ops[d]
            if o.eng == eng and not o.is_dma and not op.is_dma:
                if eng == "pe":
                    continue
                if d not in raw:
                    continue
            deps.add(d)
        op.deps = deps
        ops.append(op)
        return op

    def barrier(self, engines, scratch_fns):
        marks = []
        for e in engines:
            m = Reg("bar_" + e)
            reads = [kr for kr in self.keyregs.values()] if e == "sp" else []
            self.add(e, scratch_fns[e], reads=reads, writes=[m])
            marks.append(m)
        for e in engines:
            self.add(e, scratch_fns[e], reads=marks, writes=[])

    def emit(self):
        nc = self.nc
        ops = self.ops
        for op in ops:
            for d in op.deps:
                ops[d].signal = True
        cnt = {}
        epoch = {}
        dmacnt = {}
        for op in ops:
            if op.is_dma:
                c = dmacnt.get(op.key, 0) + 16 * op.ninc
                dmacnt[op.key] = c
                op.sig = (("dma", op.key), c)
            elif op.signal:
                e = op.eng
                c = cnt.get(e, 0) + 1
                if c > 30000:
                    epoch[e] = epoch.get(e, 0) + 1
                    c = 1
                cnt[e] = c
                op.sig = ((e, epoch.get(e, 0)), c)
        sems = {}

        def sem(k):
            s = sems.get(k)
            if s is None:
                s = sems[k] = nc.alloc_semaphore("s%d" % len(sems))
            return s

        for op in ops:
            if op.sig is not None:
                sem(op.sig[0])
        per = {}
        for op in ops:
            per.setdefault(op.eng, []).append(op)
        final_dma = dict(dmacnt)

        self.icount = {}

        def run(engname, eng):
            waited = {}
            nwait = [0]
            for op in per.get(engname, []):
                w = {}
                for d in op.deps:
                    k, v = ops[d].sig
                    if w.get(k, 0) < v:
                        w[k] = v
                for k, v in w.items():
                    if waited.get(k, 0) < v:
                        eng.wait_ge(sems[k], v)
                        waited[k] = v
                        nwait[0] += 1
                ins = op.fn(eng)
                if op.is_dma:
                    if not isinstance(ins, (list, tuple)):
                        ins = [ins]
                    assert len(ins) == op.ninc
                    for i_ in ins:
                        i_.then_inc(sems[op.sig[0]], 16)
                elif op.signal:
                    ins.then_inc(sems[op.sig[0]], 1)
            self.icount[engname] = self.icount.get(engname, 0) + len(per.get(engname, [])) + nwait[0]
            if engname == "sp":
                for key, c in final_dma.items():
                    k = ("dma", key)
                    if waited.get(k, 0) < c:
                        eng.wait_ge(sems[k], c)

        with nc.Block() as block:
            @block.tensor
            def _(e):
                run("pe", e)

            @block.scalar
            def _(e):
                run("act", e)

            @block.vector
            def _(e):
                run("dve", e)

            @block.gpsimd
            def _(e):
                run("pool", e)

            @block.sync
            def _(e):
                run("sp", e)
        print("sched: %d ops, %d sems, max dma sem %d, engine counts %s epochs %s" % (
            len(ops), len(sems), max(dmacnt.values()) if dmacnt else 0, cnt, epoch))
        print("instr per engine (ops+waits):", self.icount)


class Builder:
    def __init__(self, S, NSEQ, layers):
        self.S = S
        self.NSEQ = NSEQ
        self.layers = layers
        self.nc = bass.Bass("TRN2", target_bir_lowering=False)
        self.T = Sched(self.nc)
        self.din = {}
        self.uid = 0
        self.scr_idx = {}
        self.scr_pool = {}
        self.scr_n = 0

    def dram_in(self, name, shape):
        t = self.nc.dram_tensor(name, list(shape), F32, kind="ExternalInput")
        self.din[name] = t
        return t

    def dram_scratch(self, name, shape, dt):
        key = (tuple(shape), str(dt))
        idx = self.scr_idx.get(key, 0)
        self.scr_idx[key] = idx + 1
        pool = self.scr_pool.setdefault(key, [])
        if idx >= len(pool):
            self.scr_n += 1
            pool.append(self.nc.dram_tensor("scr%d" % self.scr_n, list(shape), dt, kind="Internal"))
        return pool[idx]

    def sb(self, stack, name, shape, dt):
        self.uid += 1
        return stack.enter_context(self.nc.sbuf_tensor("%s_%d" % (name, self.uid), list(shape), dt))

    def ps(self, stack, name, shape, dt=F32):
        self.uid += 1
        full = 512 if dt == F32 else 1024
        assert len(shape) == 2 and shape[1] <= full
        t = stack.enter_context(self.nc.psum_tensor("%s_%d" % (name, self.uid), [128, full], dt))
        return t[0:shape[0], 0:shape[1]]

    def dma(self, key, out, in_, reads=(), writes=()):
        return self.T.add("sp", lambda e: e.dma_start(out=out, in_=in_), reads=reads, writes=writes, key=key)

    def act(self, out, in_, func, reads, writes, bias=None, scale=None, accum_out=None):
        kw = {}
        if bias is not None:
            kw["bias"] = bias
        if scale is not None:
            kw["scale"] = scale
        if accum_out is not None:
            kw["accum_out"] = accum_out
        return self.T.add("act", lambda e: e.activation(out=out, in_=in_, func=func, **kw), reads=reads, writes=writes)

    def mm(self, out, lhsT, rhs, start, stop, reads, writes):
        return self.T.add("pe", lambda e: e.matmul(out, lhsT, rhs, start=start, stop=stop), reads=reads, writes=writes)

    def tr(self, out, in_, ident, reads, writes):
        return self.T.add("pe", lambda e: e.transpose(out, in_, ident), reads=reads, writes=writes)

    def v(self, eng, name, reads, writes, *a, **kw):
        return self.T.add(eng, lambda e: getattr(e, name)(*a, **kw), reads=reads, writes=writes)

    def rsqrt(self, out, in_, eps, reads, writes, tmp=None, r_tmp=None):
        if tmp is None:
            tmp, r_tmp = out, writes[0]
        self.act(tmp, in_, AF.Sqrt, reads, [r_tmp], bias=self.eps_ap(eps, tmp.shape[0]))
        return self.v("dve", "reciprocal", [r_tmp], writes, out=out, in_=tmp)

    def eps_ap(self, eps, p):
        return self.epsc[eps][0:p, 0:1]

    def barrier(self):
        nc = self.nc
        sc = self.bar_sc
        fns = {
            "pe": lambda e: e.matmul(self.bar_ps[0:1, 0:1], sc[0:1, 0:1], sc[0:1, 0:1], start=True, stop=True),
            "act": lambda e: e.copy(out=sc[0:1, 2:3], in_=sc[0:1, 1:2]),
            "dve": lambda e: e.memset(sc[0:1, 3:4], 0.0),
            "pool": lambda e: e.memset(sc[0:1, 4:5], 0.0),
            "sp": lambda e: e.nop(),
        }
        self.T.barrier(["pe", "act", "dve", "pool", "sp"], fns)

    def setup_consts(self, stack):
        nc, T = self.nc, self.T
        self.bar_sc = self.sb(stack, "barsc", [1, 8], BF16)
        self.bar_ps = self.ps(stack, "barps", [1, 8], F32)
        self.r_const = Reg("const")
        c_d = self.dram_in("c_ident", [128, 128])
        self.ident_f = self.sb(stack, "identf", [128, 128], F32)
        self.ident_b = self.sb(stack, "identb", [128, 128], BF16)
        self.ones_b = self.sb(stack, "onesb", [128, 128], BF16)
        self.ones_f = self.sb(stack, "onesf", [128, 128], F32)
        self.blk64 = self.sb(stack, "blk64", [128, 128], BF16)
        self.dma("c0", self.ident_f[:], c_d.ap(), writes=[self.r_const])
        self.epsc = {}
        for eps in (1e-6, 64e-5, 1.0):
            t_ = self.sb(stack, "eps", [128, 1], F32)
            T.add("dve", lambda e, t_=t_, eps=eps: e.memset(t_[:], eps), writes=[self.r_const])
            self.epsc[eps] = t_
        T.add("dve", lambda e: e.memset(self.bar_sc[:], 0.0), writes=[self.r_const])
        T.add("dve", lambda e: e.tensor_copy(out=self.ident_b[:], in_=self.ident_f[:]), reads=[self.r_const], writes=[self.r_const])
        T.add("dve", lambda e: e.memset(self.ones_b[:], 1.0), writes=[self.r_const])
        T.add("dve", lambda e: e.memset(self.ones_f[:], 1.0), writes=[self.r_const])
        T.add("dve", lambda e: e.memset(self.blk64[:], 0.0), writes=[self.r_const])
        T.add("dve", lambda e: e.memset(self.blk64[0:64, 0:64], 1.0 / 64), reads=[self.r_const], writes=[self.r_const])
        T.add("dve", lambda e: e.memset(self.blk64[64:128, 64:128], 1.0 / 64), reads=[self.r_const], writes=[self.r_const])

    def phase_hT(self, x_ap, g_ap, hT, hT_regs, norm=True):
        nc, T, S = self.nc, self.T, self.S
        nt = S // 128
        with ExitStack() as st:
            NB = 2
            xdt = F32 if norm else BF16
            xt = [self.sb(st, "xt", [128, D], xdt) for _ in range(NB)]
            r_xt = [Reg() for _ in range(NB)]
            if norm:
                gt = self.sb(st, "gt", [128, D], F32)
                r_gt = Reg()
                self.dma("gt", gt[:], g_ap.partition_broadcast(128), writes=[r_gt])
                junk = self.sb(st, "junk", [128, D], BF16)
                r_junk = Reg()
                ss = [self.sb(st, "ss", [128, 1], F32) for _ in range(NB)]
                rs = [self.sb(st, "rs", [128, 1], F32) for _ in range(NB)]
                r_ss = [Reg() for _ in range(NB)]
                r_rs = [Reg() for _ in range(NB)]
                hn = [self.sb(st, "hn", [128, D], BF16) for _ in range(NB)]
                r_hn = [Reg() for _ in range(NB)]
            tp = [self.ps(st, "tp", [128, 512], BF16) for _ in range(4)]
            r_tp = [Reg() for _ in range(4)]
            for i in range(nt):
                b = i % NB
                self.dma("xt%d" % b, xt[b][:], x_ap[i * 128:(i + 1) * 128, :], writes=[r_xt[b]])
                if norm:
                    self.act(junk[:], xt[b][:], AF.Square, [r_xt[b]], [r_junk, r_ss[b]],
                             scale=float(D ** -0.5), accum_out=ss[b][:])
                    self.rsqrt(rs[b][:], ss[b][:], 1e-6, [r_ss[b]], [r_rs[b]])
                    self.v("dve", "scalar_tensor_tensor", [r_xt[b], r_rs[b], r_gt], [r_hn[b]],
                           out=hn[b][:], in0=xt[b][:], scalar=rs[b][:, 0:1], in1=gt[:],
                           op0=ALU.mult, op1=ALU.mult)
                    src, r_src = hn[b], r_hn[b]
                else:
                    src, r_src = xt[b], r_xt[b]
                for g in range(4):
                    pb = (i * 4 + g) % 4
                    for j in range(4):
                        k = g * 4 + j
                        self.tr(tp[pb][:, j * 128:(j + 1) * 128], src[:, k * 128:(k + 1) * 128],
                                self.ident_b[:], [r_src, self.r_const], [r_tp[pb]])
                    o = hT[:, g * 4:(g + 1) * 4, i * 128:(i + 1) * 128]
                    s_ = tp[pb][:].rearrange("p (k t) -> p k t", k=4)
                    if g % 2 == 0:
                        self.v("dve", "tensor_copy", [r_tp[pb]], [hT_regs[i][g]], out=o, in_=s_)
                    else:
                        self.T.add("act", lambda e, o=o, s_=s_: e.copy(out=o, in_=s_), reads=[r_tp[pb]], writes=[hT_regs[i][g]])
        self.barrier()

    def phase_proj(self, hT, hT_regs, jobs):
        nc, T, S = self.nc, self.T, self.S
        nt = S // 128
        with ExitStack() as st:
            stage = [self.sb(st, "wst", [128, 4, 512], F32) for _ in range(2)]
            r_stage = [Reg() for _ in range(2)]
            wbf = [self.sb(st, "wbf", [128, KC, 512], BF16) for _ in range(2)]
            r_wbf = [[Reg() for _ in range(4)] for _ in range(2)]
            NPS = 3
            pss = [self.ps(st, "pj", [128, 512], F32) for _ in range(NPS)]
            r_ps = [Reg() for _ in range(NPS)]
            pcount = 0
            scount = 0
            for ji, (W_ap, n, orient, cb) in enumerate(jobs):
                wb = ji % 2
                Wv = W_ap.rearrange("(k p) n -> p k n", p=128)
                for qt in range(4):
                    sbi = scount % 2
                    scount += 1
                    self.dma("wst%d" % sbi, stage[sbi][:, :, :n], Wv[:, qt * 4:(qt + 1) * 4, :],
                             writes=[r_stage[sbi]])
                    self.v("pool", "tensor_copy", [r_stage[sbi]], [r_wbf[wb][qt]],
                           out=wbf[wb][:, qt * 4:(qt + 1) * 4, :n], in_=stage[sbi][:, :, :n])
                if orient == "tok":
                    for i in range(nt):
                        p = pcount % NPS
                        pcount += 1
                        for k in range(KC):
                            self.mm(pss[p][:, :n], hT[:, k, i * 128:(i + 1) * 128], wbf[wb][:, k, :n],
                                    k == 0, k == KC - 1,
                                    [hT_regs[i][k // 4], r_wbf[wb][k // 4]], [r_ps[p]])
                        cb(pss[p][:, :n], r_ps[p], i)
                else:
                    nfb = (n + 127) // 128
                    for fb in range(nfb):
                        fn = min(128, n - fb * 128)
                        for tg in range(S // 512):
                            p = pcount % NPS
                            pcount += 1
                            for k in range(KC):
                                rr = [hT_regs[tg * 4 + q][k // 4] for q in range(4)]
                                self.mm(pss[p][:fn, :], wbf[wb][:, k, fb * 128:fb * 128 + fn],
                                        hT[:, k, tg * 512:(tg + 1) * 512], k == 0, k == KC - 1,
                                        rr + [r_wbf[wb][k // 4]], [r_ps[p]])
                            cb(pss[p][:fn, :], r_ps[p], fb, tg, fn)
        self.barrier()

    def qknorm_cb(self, st, gain_col, dst_T, key):
        sq = [self.sb(st, "sq", [128, 512], BF16) for _ in range(2)]
        r_sq = [Reg(), Reg()]
        ssp = [self.ps(st, "ssp", [128, 512], F32) for _ in range(2)]
        r_ssp = [Reg(), Reg()]
        rstd = [self.sb(st, "rstd", [128, 512], F32) for _ in range(2)]
        r_rstd = [Reg(), Reg()]
        qn = [self.sb(st, "qn", [128, 512], BF16) for _ in range(2)]
        r_qn = [Reg(), Reg()]
        cnt = [0]

        def cb(ps, r_ps, fb, tg, fn):
            b = cnt[0] % 2
            cnt[0] += 1
            self.act(sq[b][:fn, :], ps, AF.Square, [r_ps], [r_sq[b]])
            self.mm(ssp[b][:fn, :], self.blk64[:fn, :fn], sq[b][:fn, :], True, True, [r_sq[b], self.r_const], [r_ssp[b]])
            self.rsqrt(rstd[b][:fn, :], ssp[b][:fn, :], 1e-6, [r_ssp[b]], [r_rstd[b]])
            self.v("dve", "scalar_tensor_tensor", [r_ps, r_rstd[b], self.r_const], [r_qn[b]],
                   out=qn[b][:fn, :], in0=ps, scalar=gain_col[:fn, 0:1], in1=rstd[b][:fn, :],
                   op0=ALU.mult, op1=ALU.mult)
            self.dma("%s%d" % (key, b), dst_T[fb * 128:fb * 128 + fn, tg * 512:(tg + 1) * 512], qn[b][:fn, :],
                     reads=[r_qn[b]])
        return cb

    def store_cb(self, st, dst, key, func=None, feat=False, col0=0, scale=None):
        ev = [self.sb(st, "ev", [128, 512], BF16) for _ in range(2)]
        r_ev = [Reg(), Reg()]
        cnt = [0]

        def go(ps, r_ps, b, dst_ap, pn, fn_):
            o = ev[b][:pn, :fn_]
            if func is None and scale is None and cnt[0] % 2 == 0:
                self.v("dve", "tensor_copy", [r_ps], [r_ev[b]], out=o, in_=ps)
            else:
                self.act(o, ps, func if func is not None else AF.Copy, [r_ps], [r_ev[b]], scale=scale)
            self.dma("%s%d" % (key, b), dst_ap, o, reads=[r_ev[b]])

        def cb_tok(ps, r_ps, i):
            b = cnt[0] % 2
            cnt[0] += 1
            n = ps.shape[1]
            go(ps, r_ps, b, dst[i * 128:(i + 1) * 128, col0:col0 + n], 128, n)

        def cb_feat(ps, r_ps, fb, tg, fn):
            b = cnt[0] % 2
            cnt[0] += 1
            go(ps, r_ps, b, dst[col0 + fb * 128:col0 + fb * 128 + fn, tg * 512:(tg + 1) * 512], fn, 512)
        return cb_feat if feat else cb_tok

    def load_col(self, st, vec_ap, n, reps, mul=None):
        t = self.sb(st, "col", [n * reps, 1], F32)
        r = Reg()
        for j in range(reps):
            self.dma("col", t[j * n:(j + 1) * n, :], vec_ap.rearrange("o n -> n o"), writes=[r])
        if mul is not None:
            self.v("dve", "tensor_scalar", [r], [r], out=t[:], in0=t[:], scalar1=float(mul), scalar2=None, op0=ALU.mult)
        return t, r

    def out_proj(self, st, zT, z_regs, w_out_ap, x_ap, xn_ap):
        xr = [self.sb(st, "xr", [128, 512], F32) for _ in range(3)]
        r_xr = [Reg() for _ in range(3)]
        cnt = [0]
        jobs = []
        for cg in range(4):
            def cb(ps, r_ps, i, cg=cg):
                b = cnt[0] % 3
                cnt[0] += 1
                self.dma("xr%d" % b, xr[b][:], x_ap[i * 128:(i + 1) * 128, cg * 512:(cg + 1) * 512], writes=[r_xr[b]])
                self.v("dve", "tensor_tensor", [r_ps, r_xr[b]], [r_xr[b]], out=xr[b][:], in0=ps, in1=xr[b][:], op=ALU.add)
                self.dma("xr%d" % b, xn_ap[i * 128:(i + 1) * 128, cg * 512:(cg + 1) * 512], xr[b][:], reads=[r_xr[b]])
            jobs.append((w_out_ap[:, cg * 512:(cg + 1) * 512], 512, "tok", cb))
        self.phase_proj(zT, z_regs, jobs)

    def load_zT(self, zT, z_regs, zT_d):
        S = self.S
        rg = [Reg() for _ in range(4)]
        for k in range(KC):
            self.dma("zt%d" % (k % 4), zT[:, k, :], zT_d[k * 128:(k + 1) * 128, :], writes=[rg[k // 4]])
        for i in range(S // 128):
            for g in range(4):
                z_regs[i][g] = rg[g]

    def new_regs(self):
        return [[Reg() for _ in range(4)] for _ in range(self.S // 128)]

    def layer_swa(self, x_ap, xn_ap, P, seq):
        nc, T, S = self.nc, self.T, self.S
        nb = S // 128
        W = P["swa_w_in"].ap()[0]
        qT_d = self.dram_scratch("swa_qT%d" % seq, [2048, S], BF16).ap()
        kT_d = self.dram_scratch("swa_kT%d" % seq, [256, S], BF16).ap()
        v_d = self.dram_scratch("swa_v%d" % seq, [S, 256], BF16).ap()
        sgT_d = self.dram_scratch("swa_sgT%d" % seq, [2048, S], BF16).ap()
        zT_d = self.dram_scratch("swa_zT%d" % seq, [2048, S], BF16).ap()
        with ExitStack() as st:
            hT = self.sb(st, "hT", [128, KC, S], BF16)
            regs = self.new_regs()
            self.phase_hT(x_ap, P["swa_norm"].ap(), hT, regs)
            with ExitStack() as st2:
                qg, _ = self.load_col(st2, P["swa_q_gain"].ap(), 64, 2, mul=0.125)
                kg, _ = self.load_col(st2, P["swa_k_gain"].ap(), 64, 2)
                cbq = self.qknorm_cb(st2, qg, qT_d, "qs")
                cbk = self.qknorm_cb(st2, kg, kT_d, "ks")
                cbv = self.store_cb(st2, v_d, "vs")
                cbg = self.store_cb(st2, sgT_d, "gs", func=AF.Silu, feat=True)
                jobs = []
                for c in range(4):
                    def cq(ps, r, fb, tg, fn, c=c):
                        cbq(ps, r, c * 4 + fb, tg, fn)
                    jobs.append((W[:, c * 512:(c + 1) * 512], 512, "feat", cq))
                jobs.append((W[:, 2048:2304], 256, "feat", cbk))
                jobs.append((W[:, 2304:2560], 256, "tok", cbv))
                for c in range(4):
                    def cg_(ps, r, fb, tg, fn, c=c):
                        cbg(ps, r, c * 4 + fb, tg, fn)
                    jobs.append((W[:, 2560 + c * 512:2560 + (c + 1) * 512], 512, "feat", cg_))
                self.phase_proj(hT, regs, jobs)
        with ExitStack() as st2:
            BT = self.swa_bias(st2, P)
            r_BT = self.r_BT
            es = self.sb(st2, "es", [64, 32], F32)
            r_es = Reg()
            self.dma("es", es[:], P["swa_sinks"].ap().partition_broadcast(64), writes=[r_es])
            self.act(es[:], es[:], AF.Exp, [r_es], [r_es])
            vt = self.sb(st2, "vt", [128, nb, 256], BF16)
            r_vt = Reg()
            self.dma("vt", vt[:], v_d.rearrange("(b p) c -> p b c", p=128), writes=[r_vt])
            q = [self.sb(st2, "q", [64, 4, S], BF16) for _ in range(2)]
            sg = [self.sb(st2, "sg", [64, 4, S], BF16) for _ in range(2)]
            kt = [self.sb(st2, "kt", [64, S], BF16) for _ in range(2)]
            ob = [self.sb(st2, "ob", [64, 4, S], BF16) for _ in range(2)]
            r_q = [Reg(), Reg()]
            r_sg = [Reg(), Reg()]
            r_kt = [Reg(), Reg()]
            r_ob = [Reg(), Reg()]
            sps = [self.ps(st2, "sps", [128, 512], F32) for _ in range(2)]
            r_sps = [Reg(), Reg()]
            ops_ = [self.ps(st2, "ops", [64, 512], F32) for _ in range(2)]
            rps = [self.ps(st2, "rps", [64, 512], F32) for _ in range(2)]
            r_ops = [Reg(), Reg()]
            r_rps = [Reg(), Reg()]
            pt = [self.sb(st2, "pt", [128, 512], BF16) for _ in range(3)]
            r_pt = [Reg() for _ in range(3)]
            den = [self.sb(st2, "den", [64, 512], F32) for _ in range(2)]
            r_den = [Reg(), Reg()]
            ot = [self.sb(st2, "ot", [64, 512], F32) for _ in range(2)]
            r_ot = [Reg(), Reg()]
            c_s = 0
            c_p = 0
            c_o = 0
            for g in range(4):
                for hq in range(2):
                    gb = (g * 2 + hq) % 2
                    f0 = g * 512 + hq * 256
                    h0 = g * 8 + hq * 4
                    self.dma("q%d" % gb, q[gb][:], qT_d[f0:f0 + 256, :].rearrange("(h d) t -> d h t", d=64), writes=[r_q[gb]])
                    self.dma("sg%d" % gb, sg[gb][:], sgT_d[f0:f0 + 256, :].rearrange("(h d) t -> d h t", d=64), writes=[r_sg[gb]])
                    self.dma("kt%d" % gb, kt[gb][:], kT_d[g * 64:(g + 1) * 64, :], writes=[r_kt[gb]])
                    def pv_(pd, g=g, gb=gb, h0=h0):
                        i, o_i, kk, kb, p_i, nk = pd
                        self.mm(ops_[o_i][:], vt[:, kb, g * 64:(g + 1) * 64], pt[p_i][:], kk == 0, kk == nk - 1,
                                [r_vt, r_pt[p_i]], [r_ops[o_i]])
                        self.mm(rps[o_i][:], self.ones_b[:, 0:64], pt[p_i][:], kk == 0, kk == nk - 1,
                                [self.r_const, r_pt[p_i]], [r_rps[o_i]])
                        if kk != nk - 1:
                            return
                        self.v("dve", "tensor_tensor", [r_rps[o_i], r_es], [r_den[o_i]],
                               out=den[o_i][:].rearrange("p (h t) -> p h t", h=4),
                               in0=rps[o_i][:].rearrange("p (h t) -> p h t", h=4),
                               in1=es[:, h0:h0 + 4].unsqueeze(2).to_broadcast([64, 4, 128]), op=ALU.add)
                        self.v("dve", "reciprocal", [r_den[o_i]], [r_den[o_i]], out=den[o_i][:], in_=den[o_i][:])
                        self.v("dve", "tensor_tensor", [r_ops[o_i], r_den[o_i]], [r_ot[o_i]],
                               out=ot[o_i][:], in0=ops_[o_i][:], in1=den[o_i][:], op=ALU.mult)
                        self.v("pool", "tensor_tensor", [r_ot[o_i], r_sg[gb]], [r_ob[gb]],
                               out=ob[gb][:, :, i * 128:(i + 1) * 128],
                               in0=ot[o_i][:].rearrange("p (h t) -> p h t", h=4),
                               in1=sg[gb][:, :, i * 128:(i + 1) * 128], op=ALU.mult)
                    pend = None
                    for i in range(nb):
                        o_i = c_o % 2
                        c_o += 1
                        kbs = [i - 1, i] if i > 0 else [i]
                        for kk, kb in enumerate(kbs):
                            blk = 0 if kb == i - 1 else 1
                            s_i = c_s % 2
                            c_s += 1
                            p_i = c_p % 3
                            c_p += 1
                            self.mm(sps[s_i][:], kt[gb][:, kb * 128:(kb + 1) * 128],
                                    q[gb][:, :, i * 128:(i + 1) * 128], True, False,
                                    [r_kt[gb], r_q[gb]], [r_sps[s_i]])
                            self.mm(sps[s_i][:], self.ident_b[:], BT[blk][:, h0:h0 + 4, :], False, True,
                                    [r_BT, self.r_const], [r_sps[s_i]])
                            self.act(pt[p_i][:], sps[s_i][:], AF.Exp, [r_sps[s_i]], [r_pt[p_i]])
                            if pend is not None:
                                pv_(pend)
                            pend = (i, o_i, kk, kb, p_i, len(kbs))
                    pv_(pend)
                    self.dma("ob%d" % gb, zT_d[f0:f0 + 256, :].rearrange("(h d) t -> d h t", d=64), ob[gb][:], reads=[r_ob[gb]])
        self.barrier()
        with ExitStack() as st2:
            zT = self.sb(st2, "zT", [128, KC, S], BF16)
            z_regs = self.new_regs()
            self.load_zT(zT, z_regs, zT_d)
            self.out_proj(st2, zT, z_regs, P["swa_w_out"].ap()[0], x_ap, xn_ap)

    def swa_bias(self, st, P):
        T = self.T
        oh = self.sb(st, "oh", [32, 128], F32)
        tab = self.sb(st, "tab", [32, 32], F32)
        A = self.sb(st, "antidiag", [128, 384], F32)
        msk = self.sb(st, "msk", [128, 2, 128], F32)
        r0 = Reg()
        self.dma("sb0", oh[:], self.din["c_onehot"].ap(), writes=[r0])
        self.dma("sb1", tab[:], P["rel_bias"].ap(), writes=[r0])
        self.dma("sb2", A[:], self.din["c_antidiag"].ap(), writes=[r0])
        self.dma("sb3", msk[:], self.din["c_swamask"].ap(), writes=[r0])
        f = self.sb(st, "fdh", [128, 32], F32)
        BT = [self.sb(st, "BT", [128, 32, 128], BF16) for _ in range(2)]
        self.r_BT = Reg()
        with ExitStack() as s2:
            fp = self.ps(s2, "fp", [128, 32], F32)
            r_fp = Reg()
            r_f = Reg()
            self.mm(fp[:], oh[:], tab[:], True, True, [r0], [r_fp])
            self.v("dve", "tensor_copy", [r_fp], [r_f], out=f[:], in_=fp[:])
            bp = [self.ps(s2, "bp", [128, 512], F32) for _ in range(2)]
            r_bp = [Reg(), Reg()]
            c = 0
            for blk in range(2):
                for q0 in range(0, 128, 16):
                    b = c % 2
                    c += 1
                    for qq in range(16):
                        qi = q0 + qq
                        off = (255 - qi) if blk == 1 else (127 - qi)
                        self.mm(bp[b][:, qq * 32:(qq + 1) * 32], A[:, off:off + 128], f[:], True, True,
                                [r0, r_f], [r_bp[b]])
                    self.v("dve", "tensor_tensor", [r_bp[b], r0], [self.r_BT],
                           out=BT[blk][:, :, q0:q0 + 16].rearrange("p h q -> p q h"),
                           in0=bp[b][:].rearrange("p (q h) -> p q h", h=32),
                           in1=msk[:, blk, q0:q0 + 16].unsqueeze(2).to_broadcast([128, 16, 32]), op=ALU.add)
        return BT

    def ls_bufs(self, st, pn, fn):
        return (self.sb(st, "ls_x", [pn, fn], F32), self.sb(st, "ls_a", [pn, fn], F32), Reg(), Reg())

    def log_sigmoid(self, bufs, out, ps, r_ps, bias_col, r_out, pn, fn):
        xs, a, r1, r2 = bufs
        self.act(xs[:], ps, AF.Identity, [r_ps, self.r_const], [r1], bias=bias_col)
        self.v("dve", "scalar_tensor_tensor", [r1], [r2], out=a[:], in0=xs[:], scalar=-1.0, in1=xs[:],
               op0=ALU.mult, op1=ALU.min)
        self.act(a[:], a[:], AF.Exp, [r2], [r2])
        self.act(a[:], a[:], AF.Ln, [r2], [r2], bias=self.eps_ap(1.0, pn))
        self.v("dve", "scalar_tensor_tensor", [r1, r2], [r_out], out=out, in0=xs[:], scalar=0.0, in1=a[:],
               op0=ALU.min, op1=ALU.subtract)

    def layer_fox(self, x_ap, xn_ap, P, seq):
        nc, T, S = self.nc, self.T, self.S
        nb = S // 128
        W = P["fox_w_in"].ap()[0]
        qT_d = self.dram_scratch("fox_qT%d" % seq, [2048, S], BF16).ap()
        kT_d = self.dram_scratch("fox_kT%d" % seq, [2048, S], BF16).ap()
        v_d = self.dram_scratch("fox_v%d" % seq, [S, 2048], BF16).ap()
        sgT_d = self.dram_scratch("fox_sgT%d" % seq, [2048, S], BF16).ap()
        zT_d = self.dram_scratch("fox_zT%d" % seq, [2048, S], BF16).ap()
        Fa_d = self.dram_scratch("fox_Fa%d" % seq, [32, 6, S], BF16).ap()
        with ExitStack() as st:
            logf = self.sb(st, "logf", [32, S], F32)
            r_logf = Reg()
            hT = self.sb(st, "hT", [128, KC, S], BF16)
            regs = self.new_regs()
            self.phase_hT(x_ap, P["fox_norm"].ap(), hT, regs)
            with ExitStack() as st2:
                qg, _ = self.load_col(st2, P["fox_q_gain"].ap(), 64, 2, mul=0.125)
                kg, _ = self.load_col(st2, P["fox_k_gain"].ap(), 64, 2)
                bf, r_bf = self.load_col(st2, P["fox_b_f"].ap(), 32, 1)
                cbq = self.qknorm_cb(st2, qg, qT_d, "qs")
                cbk = self.qknorm_cb(st2, kg, kT_d, "ks")
                cbg = self.store_cb(st2, sgT_d, "gs", func=AF.Silu, feat=True)
                jobs = []
                for c in range(4):
                    def cq(ps, r, fb, tg, fn, c=c):
                        cbq(ps, r, c * 4 + fb, tg, fn)
                    jobs.append((W[:, c * 512:(c + 1) * 512], 512, "feat", cq))
                for c in range(4):
                    def ck(ps, r, fb, tg, fn, c=c):
                        cbk(ps, r, c * 4 + fb, tg, fn)
                    jobs.append((W[:, 2048 + c * 512:2048 + (c + 1) * 512], 512, "feat", ck))
                for c in range(4):
                    jobs.append((W[:, 4096 + c * 512:4096 + (c + 1) * 512], 512, "tok",
                                 self.store_cb(st2, v_d, "vs", col0=c * 512)))
                for c in range(4):
                    def cg_(ps, r, fb, tg, fn, c=c):
                        cbg(ps, r, c * 4 + fb, tg, fn)
                    jobs.append((W[:, 6144 + c * 512:6144 + (c + 1) * 512], 512, "feat", cg_))

                lsb = self.ls_bufs(st2, 32, 512)

                def cf(ps, r, fb, tg, fn):
                    self.log_sigmoid(lsb, logf[:, tg * 512:(tg + 1) * 512], ps, r, bf[:, 0:1], r_logf, 32, 512)
                jobs.append((W[:, 8192:8224], 32, "feat", cf))
                self.phase_proj(hT, regs, jobs)
            with ExitStack() as st2:
                fab = (self.sb(st2, "fo", [32, S], F32), self.sb(st2, "fF", [32, S], F32),
                       self.sb(st2, "ft", [32, S], F32), self.sb(st2, "fa", [32, 6, S], BF16))
                self.fox_faug(fab, logf, r_logf, Fa_d)
            self.barrier()
        with ExitStack() as st2:
            cm_f = self.sb(st2, "cmf", [128, 128], F32)
            cm = self.sb(st2, "cm", [128, 128], BF16)
            r_cm = Reg()
            self.dma("cm", cm_f[:], self.din["c_causal"].ap(), writes=[r_cm])
            self.v("dve", "tensor_copy", [r_cm], [r_cm], out=cm[:], in_=cm_f[:])
            Qa = [self.sb(st2, "Qa", [128, S], BF16) for _ in range(2)]
            Ka = [self.sb(st2, "Ka", [128, S], BF16) for _ in range(2)]
            r_Qa = [Reg(), Reg()]
            r_Ka = [Reg(), Reg()]
            for b in range(2):
                self.v("pool", "memset", [], [r_Qa[b]], Qa[b][:], 0.0)
                self.v("pool", "memset", [], [r_Qa[b]], Qa[b][96:128, :], 1.0)
                self.v("pool", "memset", [], [r_Ka[b]], Ka[b][:], 0.0)
                self.v("pool", "memset", [], [r_Ka[b]], Ka[b][64:96, :], 1.0)
            vt = [self.sb(st2, "vt", [128, nb, 512], BF16) for _ in range(2)]
            r_vt = [Reg(), Reg()]
            sg = [self.sb(st2, "sg", [64, S], BF16) for _ in range(2)]
            ob = [self.sb(st2, "ob", [64, S], BF16) for _ in range(2)]
            r_sg = [Reg(), Reg()]
            r_ob = [Reg(), Reg()]
            sps = [self.ps(st2, "sps", [128, 512], F32) for _ in range(2)]
            r_sps = [Reg(), Reg()]
            ops_ = [self.ps(st2, "ops", [64, 512], F32) for _ in range(2)]
            rps = [self.ps(st2, "rps", [64, 512], F32) for _ in range(2)]
            r_ops = [Reg(), Reg()]
            r_rps = [Reg(), Reg()]
            pt = [self.sb(st2, "pt", [128, 512], BF16) for _ in range(3)]
            r_pt = [Reg() for _ in range(3)]
            den = [self.sb(st2, "den", [64, 512], F32) for _ in range(2)]
            r_den = [Reg(), Reg()]
            ot = [self.sb(st2, "ot", [64, 512], F32) for _ in range(2)]
            r_ot = [Reg(), Reg()]
            c_s = c_p = c_o = 0
            for h in range(32):
                hb = h % 2
                if h % 8 == 0:
                    vb = (h // 8) % 2
                    self.dma("vt%d" % vb, vt[vb][:], v_d[:, (h // 8) * 512:(h // 8 + 1) * 512].rearrange("(b p) c -> p b c", p=128),
                             writes=[r_vt[vb]])
                self.dma("qa%d" % hb, Qa[hb][0:64, :], qT_d[h * 64:(h + 1) * 64, :], writes=[r_Qa[hb]])
                self.dma("qa%d" % hb, Qa[hb][64:67, :], Fa_d[h, 0:3, :], writes=[r_Qa[hb]])
                self.dma("ka%d" % hb, Ka[hb][0:64, :], kT_d[h * 64:(h + 1) * 64, :], writes=[r_Ka[hb]])
                self.dma("ka%d" % hb, Ka[hb][96:99, :], Fa_d[h, 3:6, :], writes=[r_Ka[hb]])
                self.dma("sg%d" % hb, sg[hb][:], sgT_d[h * 64:(h + 1) * 64, :], writes=[r_sg[hb]])
                hc = (h % 8) * 64
                for sblk in range(S // 512):
                    o_i = c_o % 2
                    c_o += 1
                    nj = 4 * sblk + 4
                    def pv_(pend, nj=nj, o_i=o_i, vb=vb, hc=hc):
                        j, p_i, n, c0 = pend
                        self.mm(ops_[o_i][:, c0:512], vt[vb][:, j, hc:hc + 64], pt[p_i][:, :n], j == 0, j == nj - 1,
                                [r_vt[vb], r_pt[p_i]], [r_ops[o_i]])
                        self.mm(rps[o_i][:, c0:512], self.ones_b[:, 0:64], pt[p_i][:, :n], j == 0, j == nj - 1,
                                [self.r_const, r_pt[p_i]], [r_rps[o_i]])
                    pend = None
                    for j in range(nj):
                        q0 = max(sblk * 512, j * 128)
                        q1 = (sblk + 1) * 512
                        n = q1 - q0
                        c0 = q0 - sblk * 512
                        s_i = c_s % 2
                        c_s += 1
                        p_i = c_p % 3
                        c_p += 1
                        diag = j >= 4 * sblk
                        self.mm(sps[s_i][:, :n], Ka[hb][:, j * 128:(j + 1) * 128], Qa[hb][:, q0:q1], True, not diag,
                                [r_Ka[hb], r_Qa[hb]], [r_sps[s_i]])
                        if diag:
                            self.mm(sps[s_i][:, 0:128], self.ident_b[:], cm[:], False, True,
                                    [r_cm, self.r_const], [r_sps[s_i]])
                        self.act(pt[p_i][:, :n], sps[s_i][:, :n], AF.Exp, [r_sps[s_i]], [r_pt[p_i]])
                        if pend is not None:
                            pv_(pend)
                        pend = (j, p_i, n, c0)
                    pv_(pend)
                    self.v("dve", "reciprocal", [r_rps[o_i]], [r_den[o_i]], out=den[o_i][:], in_=rps[o_i][:])
                    self.v("dve", "tensor_tensor", [r_ops[o_i], r_den[o_i]], [r_ot[o_i]],
                           out=ot[o_i][:], in0=ops_[o_i][:], in1=den[o_i][:], op=ALU.mult)
                    self.v("pool", "tensor_tensor", [r_ot[o_i], r_sg[hb]], [r_ob[hb]],
                           out=ob[hb][:, sblk * 512:(sblk + 1) * 512], in0=ot[o_i][:],
                           in1=sg[hb][:, sblk * 512:(sblk + 1) * 512], op=ALU.mult)
                self.dma("ob%d" % hb, zT_d[h * 64:(h + 1) * 64, :], ob[hb][:], reads=[r_ob[hb]])
        self.barrier()
        with ExitStack() as st2:
            zT = self.sb(st2, "zT", [128, KC, S], BF16)
            z_regs = self.new_regs()
            self.load_zT(zT, z_regs, zT_d)
            self.out_proj(st2, zT, z_regs, P["fox_w_out"].ap()[0], x_ap, xn_ap)

    def fox_faug(self, bufs, logf, r_logf, Fa_d):
        S = self.S
        ones, F, t32, Fa = bufs
        r = Reg()
        ra = Reg()
        self.v("dve", "memset", [], [r], ones[:], 1.0)
        self.v("dve", "tensor_tensor_scan", [r, r_logf], [r], out=F[:], data0=ones[:], data1=logf[:], initial=0.0,
               op0=ALU.mult, op1=ALU.add)
        for p in range(3):
            self.v("dve", "tensor_copy", [r], [ra], out=Fa[:, p, :], in_=F[:])
            if p < 2:
                self.v("dve", "tensor_copy", [ra], [r], out=t32[:], in_=Fa[:, p, :])
                self.v("dve", "tensor_tensor", [r], [r], out=F[:], in0=F[:], in1=t32[:], op=ALU.subtract)
        self.v("dve", "tensor_scalar", [ra], [ra], out=Fa[:, 3:6, :], in0=Fa[:, 0:3, :], scalar1=-1.0, scalar2=None,
               op0=ALU.mult)
        self.dma("fa", Fa_d, Fa[:], reads=[ra])

    def layer_mlstm(self, x_ap, xn_ap, P, seq):
        nc, T, S = self.nc, self.T, self.S
        nb = S // 128
        NG = nb * 8
        W = P["mlstm_w_in"].ap()[0]
        qT_d = self.dram_scratch("ml_qT%d" % seq, [1024, S], BF16).ap()
        kT_d = self.dram_scratch("ml_kT%d" % seq, [1024, S], BF16).ap()
        k_d = self.dram_scratch("ml_k%d" % seq, [S, 1024], BF16).ap()
        v_d = self.dram_scratch("ml_v%d" % seq, [S, 2048], BF16).ap()
        so_d = self.dram_scratch("ml_so%d" % seq, [S, 2048], BF16).ap()
        sg_d = self.dram_scratch("ml_sg%d" % seq, [S, 2048], BF16).ap()
        z_d = self.dram_scratch("ml_z%d" % seq, [S, 2048], BF16).ap()
        with ExitStack() as st0:
            G = self.sb(st0, "G", [128, nb, 16], F32)
            r_G = Reg()
            with ExitStack() as st:
                hT = self.sb(st, "hT", [128, KC, S], BF16)
                regs = self.new_regs()
                self.phase_hT(x_ap, P["mlstm_norm"].ap(), hT, regs)
                with ExitStack() as st2:
                    jobs = []
                    cbq = self.store_cb(st2, qT_d, "qs", feat=True, scale=float(128 ** -0.5))
                    cbkT = self.store_cb(st2, kT_d, "ks", feat=True)
                    for c in range(2):
                        def cq(ps, r, fb, tg, fn, c=c):
                            cbq(ps, r, c * 4 + fb, tg, fn)
                        jobs.append((W[:, c * 512:(c + 1) * 512], 512, "feat", cq))
                    for c in range(2):
                        def ck(ps, r, fb, tg, fn, c=c):
                            cbkT(ps, r, c * 4 + fb, tg, fn)
                        jobs.append((W[:, 1024 + c * 512:1024 + (c + 1) * 512], 512, "feat", ck))
                        jobs.append((W[:, 1024 + c * 512:1024 + (c + 1) * 512], 512, "tok",
                                     self.store_cb(st2, k_d, "kk", col0=c * 512)))
                    for c in range(4):
                        jobs.append((W[:, 2048 + c * 512:2048 + (c + 1) * 512], 512, "tok",
                                     self.store_cb(st2, v_d, "vs", col0=c * 512)))
                    for c in range(4):
                        jobs.append((W[:, 4096 + c * 512:4096 + (c + 1) * 512], 512, "tok",
                                     self.store_cb(st2, so_d, "so", func=AF.Sigmoid, col0=c * 512)))
                    for c in range(4):
                        jobs.append((W[:, 6144 + c * 512:6144 + (c + 1) * 512], 512, "tok",
                                     self.store_cb(st2, sg_d, "sgs", func=AF.Silu, col0=c * 512)))

                    def cif(ps, r, i):
                        self.v("dve", "tensor_copy", [r], [r_G], out=G[:, i, :], in_=ps)
                    jobs.append((W[:, 8192:8208], 16, "tok", cif))
                    self.phase_proj(hT, regs, jobs)
            with ExitStack() as st2:
                def t_(name, shape, dt=F32):
                    return self.sb(st2, name, shape, dt)
                bi = t_("bi", [128, 8])
                bfv = t_("bfv", [128, 8])
                r_b = Reg()
                self.dma("mlb", bi[:], P["mlstm_b_i"].ap().partition_broadcast(128), writes=[r_b])
                self.dma("mlb", bfv[:], P["mlstm_b_f"].ap().partition_broadcast(128), writes=[r_b])
                triu = t_("triu", [128, 128])
                r_tri = Reg()
                self.dma("mlt", triu[:], self.din["c_triu"].ap(), writes=[r_tri])
                gain = t_("gain", [128, 2048])
                r_gain = Reg()
                self.dma("mlg", gain[:], P["mlstm_h_gain"].ap().rearrange("o h v -> o (h v)").partition_broadcast(128), writes=[r_gain])
                il = t_("il", [128, nb, 8]); fx = t_("fx", [128, nb, 8]); fa = t_("fa", [128, nb, 8]); fl = t_("fl", [128, nb, 8])
                bb = t_("bb", [128, NG]); u = t_("u", [128, NG]); E1 = t_("E1", [128, NG]); thr = t_("thr", [128, NG])
                apB = t_("apB", [128, 2 * NG])
                Ucol = t_("Ucol", [128, 1])
                rows = t_("rows", [1, 6, NG])
                r_g = Reg()
                bc = lambda tl: tl[:].unsqueeze(1).to_broadcast([128, nb, 8])
                self.v("dve", "tensor_tensor", [r_G, r_b], [r_g], out=il[:], in0=G[:, :, 0:8], in1=bc(bi), op=ALU.add)
                self.v("dve", "tensor_tensor", [r_G, r_b], [r_g], out=fx[:], in0=G[:, :, 8:16], in1=bc(bfv), op=ALU.add)
                self.v("dve", "scalar_tensor_tensor", [r_g], [r_g], out=fa[:], in0=fx[:], scalar=-1.0, in1=fx[:], op0=ALU.mult, op1=ALU.min)
                self.act(fa[:], fa[:], AF.Exp, [r_g], [r_g])
                self.act(fa[:], fa[:], AF.Ln, [r_g], [r_g], bias=self.eps_ap(1.0, 128))
                self.v("dve", "scalar_tensor_tensor", [r_g], [r_g], out=fl[:], in0=fx[:], scalar=0.0, in1=fa[:], op0=ALU.min, op1=ALU.subtract)
                flf = fl[:].rearrange("p c h -> p (c h)")
                ilf = il[:].rearrange("p c h -> p (c h)")
                with ExitStack() as st3:
                    pA = self.ps(st3, "pA", [128, 2 * NG], F32)
                    pB = self.ps(st3, "pB", [128, 128], F32)
                    pR = self.ps(st3, "pR", [1, 2 * NG], F32)
                    r_pA, r_pB, r_pR = Reg(), Reg(), Reg()
                    self.mm(pA[:, 0:NG], triu[:], flf, True, True, [r_tri, r_g], [r_pA])
                    self.v("dve", "tensor_copy", [r_pA], [r_g], out=bb[:], in_=pA[:, 0:NG])
                    self.v("dve", "tensor_tensor", [r_g], [r_g], out=u[:], in0=ilf, in1=bb[:], op=ALU.subtract)
                    self.mm(pR[:, 0:NG], self.ones_f[:, 0:1], flf, True, True, [self.r_const, r_g], [r_pR])
                    self.v("dve", "tensor_copy", [r_pR], [r_g], out=rows[:, 1, :], in_=pR[:, 0:NG])
                    self.tr(pB[:NG, :], u[:], self.ident_f[:], [r_g, self.r_const], [r_pB])
                    self.v("dve", "reduce_max", [r_pB], [r_g], out=Ucol[:NG, :], in_=pB[:NG, :], axis=AX.X)
                    self.mm(pR[:, NG:2 * NG], Ucol[:NG, :], self.ident_f[:NG, :NG], True, True, [r_g, self.r_const], [r_pR])
                    self.v("dve", "tensor_copy", [r_pR], [r_g], out=rows[:, 0, :], in_=pR[:, NG:2 * NG])
                    rv = lambda j: rows[:, j, :].rearrange("p (c h) -> p c h", h=8)
                    for c in range(nb):
                        cs = slice(c * 8, (c + 1) * 8)
                        if c == 0:
                            self.v("dve", "tensor_scalar", [r_g], [r_g], out=rows[:, 2, cs], in0=rows[:, 0, cs], scalar1=0.0,
                                   scalar2=None, op0=ALU.max)
                        else:
                            self.v("dve", "tensor_tensor", [r_g], [r_g], out=rows[:, 2, cs], in0=rows[:, 0, cs],
                                   in1=rows[:, 2, (c - 1) * 8:c * 8], op=ALU.max)
                        self.v("dve", "tensor_tensor", [r_g], [r_g], out=rows[:, 2, cs], in0=rows[:, 2, cs],
                               in1=rows[:, 1, cs], op=ALU.add)
                    self.v("dve", "tensor_tensor", [r_g], [r_g], out=rows[:, 3, :], in0=rows[:, 2, :], in1=rows[:, 1, :], op=ALU.subtract)
                    self.v("dve", "memset", [r_g], [r_g], rows[:, 5, :], 0.0)
                    if nb > 1:
                        self.v("dve", "tensor_copy", [r_g], [r_g], out=rows[:, 5, 8:NG], in_=rows[:, 2, 0:NG - 8])
                    self.v("dve", "tensor_tensor", [r_g], [r_g], out=rows[:, 4, :], in0=rows[:, 5, :], in1=rows[:, 3, :], op=ALU.subtract)
                    self.act(rows[:, 4, :], rows[:, 4, :], AF.Exp, [r_g], [r_g])
                    self.mm(pA[:], self.ones_f[0:1, :], rows[:, 3:5, :].rearrange("p a n -> p (a n)"), True, True,
                            [self.r_const, r_g], [r_pA])
                    self.v("dve", "tensor_copy", [r_pA], [r_g], out=apB[:], in_=pA[:])
                    self.v("dve", "tensor_tensor", [r_g], [r_g], out=E1[:], in0=u[:], in1=apB[:, 0:NG], op=ALU.subtract)
                    self.act(E1[:], E1[:], AF.Exp, [r_g], [r_g])
                    self.v("dve", "tensor_tensor", [r_g], [r_g], out=thr[:], in0=bb[:], in1=apB[:, 0:NG], op=ALU.add)
                    self.act(thr[:], thr[:], AF.Exp, [r_g], [r_g], scale=-1.0)
                lam = apB[:, NG:2 * NG]
                qT = self.sb(st2, "qT", [128, 8, S], BF16)
                kT = self.sb(st2, "kT", [128, 8, S], BF16)
                ktok = self.sb(st2, "ktok", [128, nb, 1024], BF16)
                r_ld = Reg()
                for h in range(8):
                    self.dma("mq%d" % (h % 2), qT[:, h, :], qT_d[h * 128:(h + 1) * 128, :], writes=[r_ld])
                    self.dma("mk%d" % (h % 2), kT[:, h, :], kT_d[h * 128:(h + 1) * 128, :], writes=[r_ld])
                self.dma("mkt", ktok[:], k_d.rearrange("(b p) c -> p b c", p=128), writes=[r_ld])
                vp = [self.sb(st2, "vp", [128, 8, 257], BF16) for _ in range(2)]
                so = [self.sb(st2, "so", [128, 2048], BF16) for _ in range(2)]
                sgt = [self.sb(st2, "sgt", [128, 2048], BF16) for _ in range(2)]
                zt = [self.sb(st2, "zt", [128, 2048], BF16) for _ in range(2)]
                r_vp = [Reg(), Reg()]; r_so = [Reg(), Reg()]; r_sgt = [Reg(), Reg()]; r_zt = [Reg(), Reg()]
                for b in range(2):
                    self.v("pool", "memset", [], [r_vp[b]], vp[b][:], 1.0)
                CT = [self.sb(st2, "CT", [128, 257], F32) for _ in range(8)]
                CTb = [self.sb(st2, "CTb", [128, 257], BF16) for _ in range(8)]
                r_CT = [Reg() for _ in range(8)]
                r_CTb = [Reg() for _ in range(8)]
                for h in range(8):
                    self.v("pool", "memset", [], [r_CT[h]], CT[h][:], 0.0)
                sps = [self.ps(st2, "sps", [128, 128], F32) for _ in range(2)]
                ndp = [self.ps(st2, "ndp", [128, 257], F32) for _ in range(2)]
                dcp = [self.ps(st2, "dcp", [128, 257], F32) for _ in range(2)]
                r_sps = [Reg(), Reg()]; r_ndp = [Reg(), Reg()]; r_dcp = [Reg(), Reg()]
                swt = [self.sb(st2, "swt", [128, 128], BF16) for _ in range(2)]
                Vs = [self.sb(st2, "Vs", [128, 257], BF16) for _ in range(2)]
                r_swt = [Reg(), Reg()]; r_Vs = [Reg(), Reg()]
                sm = [self.sb(st2, "sm", [128, 8], F32) for _ in range(2)]
                r_sm = [Reg(), Reg()]
                hh = [self.sb(st2, "hh", [128, 256], F32) for _ in range(2)]
                hj = self.sb(st2, "hj", [128, 256], BF16)
                r_hh = [Reg(), Reg()]
                r_hj = Reg()
                cc = 0
                for c in range(nb):
                    cb_ = c % 2
                    tok = slice(c * 128, (c + 1) * 128)
                    self.dma("mv%d" % cb_, vp[cb_][:, :, 0:256], v_d[tok, :].rearrange("p (h v) -> p h v", h=8), writes=[r_vp[cb_]])
                    self.dma("mso%d" % cb_, so[cb_][:], so_d[tok, :], writes=[r_so[cb_]])
                    self.dma("msg%d" % cb_, sgt[cb_][:], sg_d[tok, :], writes=[r_sgt[cb_]])
                    self.v("pool", "tensor_tensor", [r_so[cb_], r_sgt[cb_]], [r_so[cb_]], out=so[cb_][:], in0=so[cb_][:], in1=sgt[cb_][:], op=ALU.mult)
                    for h in range(8):
                        b = cc % 2
                        cc += 1
                        col = c * 8 + h
                        self.mm(sps[b][:], kT[:, h, tok], qT[:, h, tok], True, True, [r_ld], [r_sps[b]])
                        self.v("dve", "tensor_tensor", [r_sps[b], r_tri], [r_swt[b]], out=swt[b][:], in0=sps[b][:], in1=triu[:], op=ALU.mult)
                        self.act(Vs[b][:], vp[cb_][:, h, :], AF.Copy, [r_vp[cb_], r_g], [r_Vs[b]], scale=E1[:, col:col + 1])
                        self.v("dve", "tensor_scalar", [r_CT[h], r_g], [r_CT[h]], out=CT[h][:], in0=CT[h][:], scalar1=lam[:, col:col + 1],
                               scalar2=0.0, op0=ALU.mult, op1=ALU.add)
                        self.v("pool", "tensor_copy", [r_CT[h]], [r_CTb[h]], out=CTb[h][:], in_=CT[h][:])
                        self.mm(ndp[b][:], qT[:, h, tok], CTb[h][:], True, False, [r_ld, r_CTb[h]], [r_ndp[b]])
                        self.mm(ndp[b][:], swt[b][:], Vs[b][:], False, True, [r_swt[b], r_Vs[b]], [r_ndp[b]])
                        self.mm(dcp[b][:], ktok[:, c, h * 128:(h + 1) * 128], Vs[b][:], True, True, [r_ld, r_Vs[b]], [r_dcp[b]])
                        self.v("dve", "tensor_tensor", [r_CT[h], r_dcp[b]], [r_CT[h]], out=CT[h][:], in0=CT[h][:], in1=dcp[b][:], op=ALU.add)
                        self.v("dve", "tensor_copy", [r_ndp[b]], [r_sm[b]], out=sm[b][:, 4:5], in_=ndp[b][:, 256:257])
                        self.v("dve", "scalar_tensor_tensor", [r_sm[b]], [r_sm[b]], out=sm[b][:, 0:1], in0=sm[b][:, 4:5], scalar=-1.0,
                               in1=sm[b][:, 4:5], op0=ALU.mult, op1=ALU.max)
                        self.v("dve", "tensor_tensor", [r_sm[b], r_g], [r_sm[b]], out=sm[b][:, 0:1], in0=sm[b][:, 0:1], in1=thr[:, col:col + 1], op=ALU.max)
                        self.v("dve", "reciprocal", [r_sm[b]], [r_sm[b]], out=sm[b][:, 1:2], in_=sm[b][:, 0:1])
                        self.act(hh[b][:], ndp[b][:, 0:256], AF.Copy, [r_ndp[b], r_sm[b]], [r_hh[b]], scale=sm[b][:, 1:2])
                        self.act(hj[:], hh[b][:], AF.Square, [r_hh[b]], [r_hj, r_sm[b]], scale=1.0 / 16, accum_out=sm[b][:, 2:3])
                        self.rsqrt(sm[b][:, 3:4], sm[b][:, 2:3], 1e-6, [r_sm[b]], [r_sm[b]])
                        self.v("dve", "scalar_tensor_tensor", [r_hh[b], r_sm[b], r_gain], [r_hh[b]], out=hh[b][:], in0=hh[b][:],
                               scalar=sm[b][:, 3:4], in1=gain[:, h * 256:(h + 1) * 256], op0=ALU.mult, op1=ALU.mult)
                        self.v("pool", "tensor_tensor", [r_hh[b], r_so[cb_]], [r_zt[cb_]], out=zt[cb_][:, h * 256:(h + 1) * 256], in0=hh[b][:],
                               in1=so[cb_][:, h * 256:(h + 1) * 256], op=ALU.mult)
                    self.dma("mz%d" % cb_, z_d[tok, :], zt[cb_][:], reads=[r_zt[cb_]])
        self.barrier()
        with ExitStack() as st2:
            zT = self.sb(st2, "zT", [128, KC, S], BF16)
            z_regs = self.new_regs()
            self.phase_hT(z_d, None, zT, z_regs, norm=False)
            self.out_proj(st2, zT, z_regs, P["mlstm_w_out"].ap()[0], x_ap, xn_ap)

    def load_cols(self, st, dst, c0, vec_ap, nrows, key):
        n = nrows * 16
        with ExitStack() as s2:
            A = self.sb(s2, "lcA", [n, 128], F32)
            pp = self.ps(s2, "lcp", [128, n], F32)
            r, rp = Reg(), Reg()
            self.dma(key, A[:], vec_ap.rearrange("r (k p) -> (r k) p", p=128), writes=[r])
            self.tr(pp[:], A[:], self.ident_f[:n, :n], [r, self.r_const], [rp])
            self.v("dve", "tensor_copy", [rp], [self.r_pc], out=dst[:, c0:c0 + n], in_=pp[:])
        self.barrier()

    def layer_rwkv(self, x_ap, xn_ap, P, seq):
        nc, T, S = self.nc, self.T, self.S
        nb = S // 128
        C = 64
        nch = S // C
        c0e = float(np.exp(-0.5))
        W4 = P["rwkv_w_in"].ap()[0]
        dsc = lambda n, shp, dt=BF16: self.dram_scratch("rw_%s%d" % (n, seq), shp, dt).ap()
        rT_d, kT_d = dsc("rT", [2048, S]), dsc("kT", [2048, S])
        v_d, sg_d, z_d = dsc("v", [S, 2048]), dsc("sg", [S, 2048]), dsc("z", [S, 2048])
        at_d, bt_d, kt_d, rt_d = dsc("at", [2048, S]), dsc("bt", [2048, S]), dsc("kt", [2048, S]), dsc("rt", [2048, S])
        y_d = dsc("y", [S, 2048], F32)
        ge_d = dsc("ge", [2048, nch], F32)
        with ExitStack() as st0:
            pc = self.sb(st0, "pc", [128, 11 * 16], F32)
            omm = self.sb(st0, "omm", [128, 96], F32)
            self.r_pc = Reg()
            self.load_cols(st0, pc, 0, P["rwkv_mix"].ap()[0], 6, "lc")
            for j, nm in enumerate(["rwkv_w0", "rwkv_a0", "rwkv_k_k", "rwkv_k_a"]):
                self.load_cols(st0, pc, 96 + j * 16, P[nm].ap(), 1, "lc")
            self.load_cols(st0, pc, 160, P["rwkv_r_k"].ap().rearrange("o h n -> o (h n)"), 1, "lc")
            self.v("dve", "tensor_scalar", [self.r_pc], [self.r_pc], out=omm[:], in0=pc[:, 0:96], scalar1=-1.0, scalar2=1.0,
                   op0=ALU.mult, op1=ALU.add)
            r_pc = self.r_pc
            PC = lambda r, k: pc[:, r * 16 + k:r * 16 + k + 1]
            t1T = self.sb(st0, "t1T", [96, S], BF16)
            a1T = self.sb(st0, "a1T", [96, S], BF16)
            r_t1, r_a1 = Reg(), Reg()
            bonus = self.sb(st0, "bonus", [128, nb, 32], F32)
            r_bonus = Reg()
            with ExitStack() as st:
                hT = self.sb(st, "hT", [128, KC, S], BF16)
                regs = self.new_regs()
                self.phase_hT(x_ap, P["rwkv_norm"].ap(), hT, regs)
                xmT = self.sb(st, "xmT", [128, KC, S], BF16)
                for cv in range(6):
                    rg = [Reg() for _ in range(4)]
                    xm_regs = [[rg[g] for g in range(4)] for _ in range(nb)]
                    for k in range(KC):
                        eng = "dve" if k % 2 == 0 else "pool"
                        hr = [regs[i][k // 4] for i in range(nb)]
                        self.v("pool", "tensor_scalar", hr + [r_pc], [rg[k // 4]], out=xmT[:, k, :], in0=hT[:, k, :],
                               scalar1=omm[:, cv * 16 + k:cv * 16 + k + 1], scalar2=0.0, op0=ALU.mult, op1=ALU.add)
                        self.v("dve", "scalar_tensor_tensor", hr + [r_pc, rg[k // 4]], [rg[k // 4]], out=xmT[:, k, 1:S],
                               in0=hT[:, k, 0:S - 1], scalar=PC(cv, k), in1=xmT[:, k, 1:S], op0=ALU.mult, op1=ALU.add)
                    with ExitStack() as st2:
                        jobs = []
                        if cv < 4:
                            if cv == 0:
                                cbx = self.store_cb(st2, rT_d, "qs", feat=True)
                            elif cv == 1:
                                cbx = self.store_cb(st2, kT_d, "ks", feat=True)
                            for c in range(4):
                                Wc = W4[cv][:, c * 512:(c + 1) * 512]
                                if cv < 2:
                                    def cf_(ps, r, fb, tg, fn, c=c, cbx=cbx):
                                        cbx(ps, r, c * 4 + fb, tg, fn)
                                    jobs.append((Wc, 512, "feat", cf_))
                                elif cv == 2:
                                    jobs.append((Wc, 512, "tok", self.store_cb(st2, v_d, "vs", col0=c * 512)))
                                else:
                                    jobs.append((Wc, 512, "tok", self.store_cb(st2, sg_d, "sgs", func=AF.Silu, col0=c * 512)))
                        elif cv == 4:
                            def cw(ps, r, fb, tg, fn):
                                self.act(t1T[:, tg * 512:(tg + 1) * 512], ps, AF.Tanh, [r], [r_t1])
                            jobs.append((P["rwkv_w_lora1"].ap()[0], 96, "feat", cw))
                        else:
                            def ca(ps, r, fb, tg, fn):
                                self.act(a1T[:, tg * 512:(tg + 1) * 512], ps, AF.Copy, [r], [r_a1])
                            jobs.append((P["rwkv_a_lora1"].ap()[0], 96, "feat", ca))
                        self.phase_proj(xmT, xm_regs, jobs)
            with ExitStack() as st:
                f32t = lambda n: self.sb(st, n, [128, S], F32)
                b16t = lambda n: self.sb(st, n, [128, S], BF16)
                l2s = self.sb(st, "l2s", [96, 2048], F32)
                wl2 = self.sb(st, "wl2", [96, 2048], BF16)
                al2 = self.sb(st, "al2", [96, 2048], BF16)
                r_l2 = Reg()
                r_l2s = Reg()
                self.dma("l2", l2s[:], P["rwkv_w_lora2"].ap()[0], writes=[r_l2s])
                self.v("dve", "tensor_copy", [r_l2s], [r_l2], out=wl2[:], in_=l2s[:])
                self.dma("l2", l2s[:], P["rwkv_a_lora2"].ap()[0], reads=[r_l2], writes=[r_l2s])
                self.v("dve", "tensor_copy", [r_l2s], [r_l2], out=al2[:], in_=l2s[:])
                rmask = f32t("rmask")
                hind = self.sb(st, "hind", [128, 2], BF16)
                blk1 = self.sb(st, "blk1", [128, 128], BF16)
                tiny = self.sb(st, "tiny", [128, 1], F32)
                r_k0 = Reg()
                self.v("pool", "memset", [], [r_k0], rmask[:], 1.0)
                self.v("pool", "memset", [r_k0], [r_k0], rmask[:].rearrange("p (c t) -> p c t", t=C)[:, :, 0:1], 0.0)
                self.v("pool", "memset", [], [r_k0], hind[:], 0.0)
                self.v("pool", "memset", [r_k0], [r_k0], hind[0:64, 0:1], 1.0)
                self.v("pool", "memset", [r_k0], [r_k0], hind[64:128, 1:2], 1.0)
                self.v("pool", "memset", [], [r_k0], blk1[:], 0.0)
                self.v("pool", "memset", [r_k0], [r_k0], blk1[0:64, 0:64], 1.0)
                self.v("pool", "memset", [r_k0], [r_k0], blk1[64:128, 64:128], 1.0)
                self.v("pool", "memset", [], [r_k0], tiny[:], 1e-24)
                sigw, aa, cums, Ep, Em, Epv = f32t("sigw"), f32t("aa"), f32t("cums"), f32t("Ep"), f32t("Em"), f32t("Epv")
                kkr, rn, kp, tt = f32t("kkr"), f32t("rn"), f32t("kp"), f32t("tt")
                rj, kj, sqb, prod = b16t("rj"), b16t("kj"), b16t("sqb"), b16t("prod")
                o_r, o_k, o_b, o_a = b16t("o_r"), b16t("o_k"), b16t("o_b"), b16t("o_a")
                gend = self.sb(st, "gend", [128, nch], F32)
                R = {n: Reg() for n in ["sigw", "aa", "cums", "Ep", "Em", "Epv", "kkr", "rn", "kp", "tt", "rj", "kj", "sqb", "prod",
                                        "o_r", "o_k", "o_b", "o_a", "gend"]}
                pd = [self.ps(st, "pd", [128, 512], F32) for _ in range(3)]
                r_pd = [Reg() for _ in range(3)]
                pbn = self.ps(st, "pbn", [128, nb * 2], F32)
                r_pbn = Reg()
                pc_ = 0
                ntg = S // 512
                for j in range(KC):
                    fs = slice(j * 128, (j + 1) * 128)
                    self.dma("drj", rj[:], rT_d[fs, :], writes=[R["rj"]])
                    self.dma("dkj", kj[:], kT_d[fs, :], writes=[R["kj"]])
                    for tg in range(ntg):
                        ts_ = slice(tg * 512, (tg + 1) * 512)
                        p = pc_ % 3
                        pc_ += 1
                        self.mm(pd[p][:], wl2[:, fs], t1T[:, ts_], True, True, [r_l2, r_t1], [r_pd[p]])
                        self.act(sigw[:, ts_], pd[p][:], AF.Sigmoid, [r_pd[p], r_pc], [R["sigw"]], bias=PC(6, j))
                        p = pc_ % 3
                        pc_ += 1
                        self.mm(pd[p][:], al2[:, fs], a1T[:, ts_], True, True, [r_l2, r_a1], [r_pd[p]])
                        self.act(aa[:, ts_], pd[p][:], AF.Sigmoid, [r_pd[p], r_pc], [R["aa"]], bias=PC(7, j))
                    self.v("dve", "tensor_tensor_scan", [r_k0, R["sigw"]], [R["cums"]], out=cums[:], data0=rmask[:], data1=sigw[:],
                           initial=0.0, op0=ALU.mult, op1=ALU.add)
                    self.act(Ep[:], cums[:], AF.Exp, [R["cums"]], [R["Ep"]], scale=-c0e)
                    self.act(Em[:], cums[:], AF.Exp, [R["cums"]], [R["Em"]], scale=c0e)
                    self.v("pool", "tensor_tensor", [R["cums"], R["sigw"]], [R["tt"]], out=tt[:], in0=cums[:], in1=sigw[:], op=ALU.subtract)
                    self.act(Epv[:], tt[:], AF.Exp, [R["tt"]], [R["Epv"]], scale=-c0e)
                    self.v("dve", "tensor_scalar", [R["kj"], r_pc], [R["kkr"]], out=kkr[:], in0=kj[:], scalar1=PC(8, j), scalar2=0.0,
                           op0=ALU.mult, op1=ALU.add)
                    self.act(sqb[:], kkr[:], AF.Square, [R["kkr"]], [R["sqb"]])
                    for tg in range(ntg):
                        ts_ = slice(tg * 512, (tg + 1) * 512)
                        p = pc_ % 3
                        pc_ += 1
                        self.mm(pd[p][:], blk1[:], sqb[:, ts_], True, True, [r_k0, R["sqb"]], [r_pd[p]])
                        self.act(rn[:, ts_], pd[p][:], AF.Sqrt, [r_pd[p], r_k0], [R["rn"]], bias=tiny[:, 0:1])
                    self.v("dve", "reciprocal", [R["rn"]], [R["rn"]], out=rn[:], in_=rn[:])
                    self.v("dve", "tensor_tensor", [R["kkr"], R["rn"]], [R["kkr"]], out=kkr[:], in0=kkr[:], in1=rn[:], op=ALU.mult)
                    self.v("pool", "tensor_scalar", [R["aa"], r_pc, R["Epv"]], [R["tt"]], out=tt[:], in0=aa[:], scalar1=-1.0, scalar2=PC(9, j),
                           op0=ALU.add, op1=ALU.mult)
                    self.v("dve", "scalar_tensor_tensor", [R["tt"], R["kj"]], [R["kp"]], out=kp[:], in0=tt[:], scalar=1.0, in1=kj[:],
                           op0=ALU.add, op1=ALU.mult)
                    self.v("dve", "tensor_tensor", [R["rj"], R["Ep"]], [R["o_r"]], out=o_r[:], in0=rj[:], in1=Ep[:], op=ALU.mult)
                    self.v("pool", "tensor_tensor", [R["kp"], R["Em"]], [R["o_k"]], out=o_k[:], in0=kp[:], in1=Em[:], op=ALU.mult)
                    self.v("dve", "tensor_tensor", [R["kkr"], R["aa"]], [R["tt"]], out=tt[:], in0=kkr[:], in1=aa[:], op=ALU.mult)
                    self.v("pool", "tensor_tensor", [R["tt"], R["Em"]], [R["o_b"]], out=o_b[:], in0=tt[:], in1=Em[:], op=ALU.mult)
                    self.v("dve", "scalar_tensor_tensor", [R["kkr"], R["Epv"]], [R["o_a"]], out=o_a[:], in0=kkr[:], scalar=-1.0, in1=Epv[:],
                           op0=ALU.mult, op1=ALU.mult)
                    self.v("pool", "tensor_copy", [R["Ep"]], [R["gend"]], out=gend[:],
                           in_=Ep[:].rearrange("p (c t) -> p c t", t=C)[:, :, C - 1])
                    self.v("dve", "scalar_tensor_tensor", [R["kp"], R["rj"], r_pc], [R["prod"]], out=prod[:], in0=kp[:], scalar=PC(10, j), in1=rj[:],
                           op0=ALU.mult, op1=ALU.mult)
                    for i in range(nb):
                        self.mm(pbn[:, i * 2:(i + 1) * 2], prod[:, i * 128:(i + 1) * 128], hind[:], True, True, [R["prod"], r_k0], [r_pbn])
                    self.v("dve", "tensor_copy", [r_pbn], [r_bonus], out=bonus[:, :, j * 2:(j + 1) * 2],
                           in_=pbn[:].rearrange("p (i h) -> p i h", h=2))
                    self.dma("dor", rt_d[fs, :], o_r[:], reads=[R["o_r"]])
                    self.dma("dok", kt_d[fs, :], o_k[:], reads=[R["o_k"]])
                    self.dma("dob", bt_d[fs, :], o_b[:], reads=[R["o_b"]])
                    self.dma("doa", at_d[fs, :], o_a[:], reads=[R["o_a"]])
                    self.dma("dge", ge_d[fs, :], gend[:], reads=[R["gend"]])
            self.barrier()
            with ExitStack() as st:
                m2f = self.sb(st, "m2f", [64, 2, 64], F32)
                mLf = self.sb(st, "mLf", [64, 64], F32)
                r_m = Reg()
                self.dma("rm0", m2f[:, 0, :], self.din["c_triu_strict"].ap()[0:64, 0:64], writes=[r_m])
                self.dma("rm0", m2f[:, 1, :], self.din["c_triu"].ap()[0:64, 0:64], writes=[r_m])
                self.dma("rm0", mLf[:], self.din["c_tril_strict"].ap()[0:64, 0:64], writes=[r_m])
                U = nch
                NG = U // 8
                HB = 2
                mk = lambda n, shp, dt: [self.sb(st, n, shp, dt) for _ in range(HB)]
                AR, BK, BKt = mk("AR", [64, U, 2, 64], BF16), mk("BK", [64, U, 2, 64], BF16), mk("BKt", [64, U, 2, 64], BF16)
                Pm, Qm, Wm = mk("Pm", [64, U, 64], F32), mk("Qm", [64, U, 64], F32), mk("Wm", [64, U, 64], F32)
                Mx = mk("Mx", [64, U, 3, 64], BF16)
                vt = mk("vt", [64, U, 64], BF16)
                Yb = mk("Yb", [64, U, 64], F32)
                gc = mk("gc", [64, U], F32)
                ST = mk("ST", [64, 64], F32)
                STb = mk("STb", [64, 64], BF16)
                Xs = mk("Xs", [64, 64], F32)
                Us = mk("Us", [64, 64], BF16)
                rr = lambda: [Reg() for _ in range(HB)]
                r_AR, r_BK, r_BKt, r_Mx, r_vt, r_Yb, r_gc, r_ST, r_STb, r_Xs, r_Us = (rr() for _ in range(11))
                r_P = [[Reg() for _ in range(NG)] for _ in range(HB)]
                r_Q = [[Reg() for _ in range(NG)] for _ in range(HB)]
                r_W = [[Reg() for _ in range(NG)] for _ in range(HB)]
                pA = [self.ps(st, "pA", [64, 512], F32) for _ in range(2)]
                pB = [self.ps(st, "pB", [64, 512], F32) for _ in range(2)]
                pT = self.ps(st, "pT", [64, 512], BF16)
                r_pA, r_pB, r_pT = [Reg(), Reg()], [Reg(), Reg()], Reg()
                sq = [self.ps(st, "sq", [64, 512], F32) for _ in range(HB)]
                r_sq = [[Reg() for _ in range(8)] for _ in range(HB)]
                ca = cbk = 0
                for hp in range(16):
                    for hh in range(HB):
                        h = hp * 2 + hh
                        fs = slice(h * 64, (h + 1) * 64)
                        cv_ = lambda ap: ap.rearrange("p (c t) -> p c t", t=C)
                        self.dma("ra%d" % hh, AR[hh][:, :, 0, :], cv_(at_d[fs, :]), writes=[r_AR[hh]])
                        self.dma("ra%d" % hh, AR[hh][:, :, 1, :], cv_(rt_d[fs, :]), writes=[r_AR[hh]])
                        self.dma("rb%d" % hh, BK[hh][:, :, 0, :], cv_(bt_d[fs, :]), writes=[r_BK[hh]])
                        self.dma("rb%d" % hh, BK[hh][:, :, 1, :], cv_(kt_d[fs, :]), writes=[r_BK[hh]])
                        self.dma("rv%d" % hh, vt[hh][:], v_d[:, fs].rearrange("(c t) f -> t c f", t=C), writes=[r_vt[hh]])
                        self.dma("rg%d" % hh, gc[hh][:], ge_d[fs, :], writes=[r_gc[hh]])
                        self.v("pool", "memset", [], [r_ST[hh]], ST[hh][:], 0.0)
                        self.v("pool", "memset", [], [r_STb[hh]], STb[hh][:], 0.0)
                        for c4 in range(0, U, 4):
                            for cc in range(4):
                                for j in range(2):
                                    sl = (cc * 2 + j) * 64
                                    self.tr(pT[:, sl:sl + 64], BK[hh][:, c4 + cc, j, :], self.ident_b[0:64, 0:64],
                                            [r_BK[hh], self.r_const], [r_pT])
                            self.v("dve", "tensor_copy", [r_pT], [r_BKt[hh]], out=BKt[hh][:, c4:c4 + 4, :, :],
                                   in_=pT[:].rearrange("p (c j t) -> p c j t", c=4, j=2))
                        for c4 in range(0, U, 4):
                            a = ca % 2
                            ca += 1
                            for cc in range(4):
                                c = c4 + cc
                                self.mm(pA[a][:, cc * 128:(cc + 1) * 128], BK[hh][:, c, 0, :], AR[hh][:, c, :, :], True, True,
                                        [r_BK[hh], r_AR[hh]], [r_pA[a]])
                            g8 = c4 // 8
                            pv = pA[a][:].rearrange("p (c j t) -> p c j t", c=4, j=2)
                            self.v("dve", "tensor_tensor", [r_pA[a], r_m], [r_P[hh][g8]], out=Pm[hh][:, c4:c4 + 4, :], in0=pv[:, :, 0, :],
                                   in1=m2f[:, 0, :].unsqueeze(1).to_broadcast([64, 4, 64]), op=ALU.mult)
                            self.v("dve", "tensor_tensor", [r_pA[a], r_m], [r_Mx[hh]], out=Mx[hh][:, c4:c4 + 4, 0, :], in0=pv[:, :, 1, :],
                                   in1=m2f[:, 1, :].unsqueeze(1).to_broadcast([64, 4, 64]), op=ALU.mult)
                            a = ca % 2
                            ca += 1
                            for cc in range(4):
                                c = c4 + cc
                                self.mm(pA[a][:, cc * 128:(cc + 1) * 128], BK[hh][:, c, 1, :], AR[hh][:, c, :, :], True, True,
                                        [r_BK[hh], r_AR[hh]], [r_pA[a]])
                            pv = pA[a][:].rearrange("p (c j t) -> p c j t", c=4, j=2)
                            self.v("dve", "tensor_tensor", [r_pA[a], r_m], [r_Mx[hh]], out=Mx[hh][:, c4:c4 + 4, 1:3, :], in0=pv,
                                   in1=m2f[:].unsqueeze(1).to_broadcast([64, 4, 2, 64]), op=ALU.mult)
                        for g8 in range(NG):
                            b_ = cbk % 2
                            cbk += 1
                            for cc in range(8):
                                c = g8 * 8 + cc
                                self.mm(pB[b_][:, cc * 64:(cc + 1) * 64], AR[hh][:, c, 0, :], BK[hh][:, c, 0, :], True, True,
                                        [r_BK[hh], r_AR[hh]], [r_pB[b_]])
                            self.v("dve", "tensor_tensor", [r_pB[b_], r_m], [r_Q[hh][g8]], out=Qm[hh][:, g8 * 8:(g8 + 1) * 8, :],
                                   in0=pB[b_][:].rearrange("p (c t) -> p c t", c=8),
                                   in1=mLf[:].unsqueeze(1).to_broadcast([64, 8, 64]), op=ALU.mult)
                            self.v("pool", "tensor_tensor", [r_P[hh][g8], self.r_const], [r_W[hh][g8]], out=Wm[hh][:, g8 * 8:(g8 + 1) * 8, :],
                                   in0=Pm[hh][:, g8 * 8:(g8 + 1) * 8, :],
                                   in1=self.ident_f[0:64, 0:64].unsqueeze(1).to_broadcast([64, 8, 64]), op=ALU.add)
                    items = [(hh, g8) for hh in range(HB) for g8 in range(NG)]
                    for step in range(5):
                        last = step == 4
                        for idx, (hh, g8) in enumerate(items):
                            us = slice(g8 * 8, (g8 + 1) * 8)
                            bP, rP = pB[idx % 2], r_pB[idx % 2]
                            bQ, rQ = pA[idx % 2], r_pA[idx % 2]
                            if not last:
                                for cc in range(8):
                                    u = g8 * 8 + cc
                                    self.mm(bP[:, cc * 64:(cc + 1) * 64], Qm[hh][:, u, :], Pm[hh][:, u, :], True, True,
                                            [r_Q[hh][g8], r_P[hh][g8]], [rP])
                            for cc in range(8):
                                u = g8 * 8 + cc
                                self.mm(bQ[:, cc * 64:(cc + 1) * 64], Pm[hh][:, u, :], Qm[hh][:, u, :], True, True,
                                        [r_Q[hh][g8], r_P[hh][g8]], [rQ])
                            if not last:
                                self.v("dve", "tensor_copy", [rP], [r_P[hh][g8]], out=Pm[hh][:, us, :],
                                       in_=bP[:].rearrange("p (c t) -> p c t", c=8))
                            self.T.add("act", lambda e, o=Qm[hh][:, us, :], i_=bQ[:].rearrange("p (c t) -> p c t", c=8): e.copy(out=o, in_=i_),
                                       reads=[rQ], writes=[r_Q[hh][g8]])
                        for idx, (hh, g8) in enumerate(items):
                            us = slice(g8 * 8, (g8 + 1) * 8)
                            bW, rW = pB[idx % 2], r_pB[idx % 2]
                            for cc in range(8):
                                u = g8 * 8 + cc
                                self.mm(bW[:, cc * 64:(cc + 1) * 64], Qm[hh][:, u, :], Wm[hh][:, u, :], True, True,
                                        [r_Q[hh][g8], r_W[hh][g8]], [rW])
                            self.v("dve", "tensor_tensor", [rW, r_W[hh][g8]], [r_W[hh][g8]], out=Wm[hh][:, us, :], in0=Wm[hh][:, us, :],
                                   in1=bW[:].rearrange("p (c t) -> p c t", c=8), op=ALU.add)
                    for c in range(U):
                        g8 = c // 8
                        o = (c % 2) * 256
                        sl = lambda hh, i: sq[hh][:, o + i * 64:o + (i + 1) * 64]
                        rb = lambda hh: r_sq[hh][0]
                        HH = range(HB)
                        for hh in HH:
                            self.mm(sl(hh, 0), AR[hh][:, c, 0, :], STb[hh][:], True, False, [r_AR[hh], r_STb[hh]], [rb(hh)])
                            self.mm(sl(hh, 0), Mx[hh][:, c, 1, :], vt[hh][:, c, :], False, True, [r_Mx[hh], r_vt[hh]], [rb(hh)])
                        for hh in HH:
                            self.T.add("act", lambda e, o_=Xs[hh][:], i_=sl(hh, 0): e.copy(out=o_, in_=i_), reads=[rb(hh)], writes=[r_Xs[hh], rb(hh)])
                        for hh in HH:
                            self.mm(sl(hh, 1), Wm[hh][:, c, :], Xs[hh][:], True, True, [r_W[hh][g8], r_Xs[hh]], [rb(hh)])
                        for hh in HH:
                            self.v("dve", "tensor_copy", [rb(hh)], [r_Us[hh], rb(hh)], out=Us[hh][:], in_=sl(hh, 1))
                        for hh in HH:
                            vm = vt[hh][:, c, :]
                            self.mm(sl(hh, 3), BKt[hh][:, c, 0, :], Us[hh][:], True, False, [r_BKt[hh], r_Us[hh]], [rb(hh)])
                            self.mm(sl(hh, 3), BKt[hh][:, c, 1, :], vm, False, True, [r_BKt[hh], r_vt[hh]], [rb(hh)])
                            self.mm(sl(hh, 2), AR[hh][:, c, 1, :], STb[hh][:], True, False, [r_AR[hh], r_STb[hh]], [rb(hh)])
                            self.mm(sl(hh, 2), Mx[hh][:, c, 0, :], Us[hh][:], False, False, [r_Mx[hh], r_Us[hh]], [rb(hh)])
                            self.mm(sl(hh, 2), Mx[hh][:, c, 2, :], vm, False, True, [r_Mx[hh], r_vt[hh]], [rb(hh)])
                        for hh in HH:
                            self.v("dve", "tensor_tensor", [rb(hh), r_ST[hh]], [r_ST[hh], rb(hh)], out=ST[hh][:], in0=ST[hh][:], in1=sl(hh, 3), op=ALU.add)
                            self.v("dve", "tensor_scalar", [r_ST[hh], r_gc[hh]], [r_ST[hh]], out=ST[hh][:], in0=ST[hh][:],
                                   scalar1=gc[hh][:, c:c + 1], scalar2=0.0, op0=ALU.mult, op1=ALU.add)
                        for hh in HH:
                            self.v("pool", "tensor_copy", [r_ST[hh]], [r_STb[hh]], out=STb[hh][:], in_=ST[hh][:])
                        for hh in HH:
                            self.T.add("act", lambda e, o_=Yb[hh][:, c, :], i_=sl(hh, 2): e.copy(out=o_, in_=i_), reads=[rb(hh)], writes=[r_Yb[hh], rb(hh)])
                    for hh in range(HB):
                        h = hp * 2 + hh
                        self.dma("ry%d" % hh, y_d[:, h * 64:(h + 1) * 64].rearrange("(c t) f -> t c f", t=C), Yb[hh][:], reads=[r_Yb[hh]])
            self.barrier()
            with ExitStack() as st:
                gw = self.sb(st, "gw", [128, 2048], F32)
                gb = self.sb(st, "gb", [128, 2048], F32)
                r_gp = Reg()
                self.dma("pg0", gw[:], P["rwkv_gn_w"].ap().partition_broadcast(128), writes=[r_gp])
                self.dma("pg1", gb[:], P["rwkv_gn_b"].ap().partition_broadcast(128), writes=[r_gp])
                NBF = 2
                yt = [self.sb(st, "yt", [128, 2048], F32) for _ in range(NBF)]
                y2 = [self.sb(st, "y2", [128, 2048], F32) for _ in range(NBF)]
                vv = [self.sb(st, "vv", [128, 2048], BF16) for _ in range(NBF)]
                sgt = [self.sb(st, "sgt", [128, 2048], BF16) for _ in range(NBF)]
                zo = [self.sb(st, "zo", [128, 2048], BF16) for _ in range(NBF)]
                sm = [self.sb(st, "sm", [128, 3, 32], F32) for _ in range(NBF)]
                r_yt, r_y2, r_vv, r_sgt, r_zo, r_sm = ([Reg() for _ in range(NBF)] for _ in range(6))
                h3 = lambda ap: ap.rearrange("p (h n) -> p h n", n=64)
                bc = lambda ap: ap.unsqueeze(2).to_broadcast([128, 32, 64])
                for i in range(nb):
                    b = i % NBF
                    tok = slice(i * 128, (i + 1) * 128)
                    self.dma("py%d" % b, yt[b][:], y_d[tok, :], writes=[r_yt[b]])
                    self.dma("pv%d" % b, vv[b][:], v_d[tok, :], writes=[r_vv[b]])
                    self.dma("ps%d" % b, sgt[b][:], sg_d[tok, :], writes=[r_sgt[b]])
                    self.v("dve", "reduce_sum", [r_yt[b]], [r_sm[b]], out=sm[b][:, 0, :], in_=h3(yt[b][:]), axis=AX.X)
                    self.v("dve", "tensor_scalar", [r_sm[b]], [r_sm[b]], out=sm[b][:, 0, :], in0=sm[b][:, 0, :], scalar1=1.0 / 64, scalar2=0.0,
                           op0=ALU.mult, op1=ALU.add)
                    self.v("dve", "tensor_tensor", [r_yt[b], r_sm[b]], [r_yt[b]], out=h3(yt[b][:]), in0=h3(yt[b][:]), in1=bc(sm[b][:, 0, :]),
                           op=ALU.subtract)
                    self.act(y2[b][:], yt[b][:], AF.Square, [r_yt[b]], [r_y2[b]], scale=0.125)
                    self.v("dve", "reduce_sum", [r_y2[b]], [r_sm[b]], out=sm[b][:, 1, :], in_=h3(y2[b][:]), axis=AX.X)
                    self.rsqrt(sm[b][:, 2, :], sm[b][:, 1, :], 64e-5, [r_sm[b]], [r_sm[b]])
                    self.v("dve", "tensor_tensor", [r_yt[b], r_sm[b]], [r_yt[b]], out=h3(yt[b][:]), in0=h3(yt[b][:]), in1=bc(sm[b][:, 2, :]),
                           op=ALU.mult)
                    self.v("pool", "tensor_tensor", [r_yt[b], r_gp], [r_yt[b]], out=yt[b][:], in0=yt[b][:], in1=gw[:], op=ALU.mult)
                    self.v("pool", "tensor_tensor", [r_yt[b], r_gp], [r_yt[b]], out=yt[b][:], in0=yt[b][:], in1=gb[:], op=ALU.add)
                    self.v("dve", "tensor_tensor", [r_vv[b], r_bonus, r_y2[b]], [r_y2[b]], out=h3(y2[b][:]), in0=h3(vv[b][:]), in1=bc(bonus[:, i, :]),
                           op=ALU.mult)
                    self.v("pool", "tensor_tensor", [r_yt[b], r_y2[b]], [r_yt[b]], out=yt[b][:], in0=yt[b][:], in1=y2[b][:], op=ALU.add)
                    self.v("dve", "tensor_tensor", [r_yt[b], r_sgt[b]], [r_zo[b]], out=zo[b][:], in0=yt[b][:], in1=sgt[b][:], op=ALU.mult)
                    self.dma("pz%d" % b, z_d[tok, :], zo[b][:], reads=[r_zo[b]])
        self.barrier()
        with ExitStack() as st2:
            zT = self.sb(st2, "zT", [128, KC, S], BF16)
            z_regs = self.new_regs()
            self.phase_hT(z_d, None, zT, z_regs, norm=False)
            self.out_proj(st2, zT, z_regs, P["rwkv_w_out"].ap()[0], x_ap, xn_ap)


PARAM_SHAPES = {
    "rel_bias": (32, 32), "swa_norm": (1, 2048), "swa_w_in": (1, 2048, 4608), "swa_q_gain": (1, 64),
    "swa_k_gain": (1, 64), "swa_sinks": (1, 32), "swa_w_out": (1, 2048, 2048),
    "rwkv_norm": (1, 2048), "rwkv_mix": (1, 6, 2048), "rwkv_w_in": (1, 4, 2048, 2048), "rwkv_w0": (1, 2048),
    "rwkv_w_lora1": (1, 2048, 96), "rwkv_w_lora2": (1, 96, 2048), "rwkv_a0": (1, 2048),
    "rwkv_a_lora1": (1, 2048, 96), "rwkv_a_lora2": (1, 96, 2048), "rwkv_k_k": (1, 2048), "rwkv_k_a": (1, 2048),
    "rwkv_r_k": (1, 32, 64), "rwkv_gn_w": (1, 2048), "rwkv_gn_b": (1, 2048), "rwkv_w_out": (1, 2048, 2048),
    "mlstm_norm": (1, 2048), "mlstm_w_in": (1, 2048, 8208), "mlstm_b_i": (1, 8), "mlstm_b_f": (1, 8),
    "mlstm_h_gain": (1, 8, 256), "mlstm_w_out": (1, 2048, 2048),
    "fox_norm": (1, 2048), "fox_w_in": (1, 2048, 8224), "fox_b_f": (1, 32), "fox_q_gain": (1, 64),
    "fox_k_gain": (1, 64), "fox_w_out": (1, 2048, 2048),
}
LAYER_PARAMS = {
    0: ["rel_bias", "swa_norm", "swa_w_in", "swa_q_gain", "swa_k_gain", "swa_sinks", "swa_w_out"],
    1: [k for k in PARAM_SHAPES if k.startswith("rwkv_")],
    2: [k for k in PARAM_SHAPES if k.startswith("mlstm_")],
    3: [k for k in PARAM_SHAPES if k.startswith("fox_")],
}


def t5_bucket_np(dist):
    max_exact = 16
    d_f = np.maximum(dist, 1).astype(np.float32)
    large = max_exact + (np.log(d_f / np.float32(max_exact)) / np.float32(np.log(128 / max_exact))
                         * np.float32(32 - max_exact)).astype(np.int32)
    large = np.minimum(large, 31)
    return np.where(dist < max_exact, dist, large)


def make_consts():
    c = {}
    c["c_ident"] = np.eye(128, dtype=np.float32)
    d = np.arange(128)
    b = t5_bucket_np(d)
    oh = np.zeros((32, 128), np.float32)
    oh[b, d] = 1.0
    c["c_onehot"] = oh
    A = np.zeros((128, 384), np.float32)
    for dd in range(128):
        A[dd, 255 - dd] = 1.0
    c["c_antidiag"] = A
    k = np.arange(128)[:, None]
    q = np.arange(128)[None, :]
    m = np.zeros((128, 2, 128), np.float32)
    m[:, 0, :] = np.where(k <= q, NEG, 0.0)
    m[:, 1, :] = np.where(k > q, NEG, 0.0)
    c["c_swamask"] = m
    c["c_causal"] = np.where(k > q, NEG, 0.0).astype(np.float32)
    c["c_triu"] = (k <= q).astype(np.float32)
    c["c_triu_strict"] = (k < q).astype(np.float32)
    c["c_tril_strict"] = (k > q).astype(np.float32)
    return c


CONST_SHAPES = {k: v.shape for k, v in make_consts().items()}


def build(S, NSEQ, layers):
    B = Builder(S, NSEQ, layers)
    nc = B.nc
    x_d = B.dram_in("x", [NSEQ * S, D])
    need = set()
    for L in layers:
        need |= set(LAYER_PARAMS[L])
    P = {name: B.dram_in(name, PARAM_SHAPES[name]) for name in PARAM_SHAPES if name in need}
    for name, shp in CONST_SHAPES.items():
        if name != "c_ident":
            B.dram_in(name, shp)
    y_d = nc.dram_tensor("y", [NSEQ * S, D], F32, kind="ExternalOutput")
    xs = [nc.dram_tensor("xs%d" % i, [NSEQ * S, D], F32, kind="Internal") for i in range(2)]
    fns = {0: B.layer_swa, 1: getattr(B, "layer_rwkv", None), 2: getattr(B, "layer_mlstm", None),
           3: getattr(B, "layer_fox", None)}
    with ExitStack() as st:
        B.setup_consts(st)
        for seq in range(NSEQ):
            cur = x_d.ap()[seq * S:(seq + 1) * S, :]
            for li, L in enumerate(layers):
                if li == len(layers) - 1:
                    nxt = y_d.ap()[seq * S:(seq + 1) * S, :]
                else:
                    nxt = xs[li % 2].ap()[seq * S:(seq + 1) * S, :]
                B.scr_idx = {}
                fns[L](cur, nxt, P, seq)
                cur = nxt
        B.T.emit()
    return nc, sorted(need)


def _launch(nc, need, xs, inputs, consts):
    in_maps = []
    for c in range(len(xs)):
        m = {"x": np.ascontiguousarray(xs[c], dtype=np.float32)}
        for name in need:
            m[name] = np.ascontiguousarray(inputs[name], dtype=np.float32)
        m.update(consts)
        in_maps.append(m)
    res = run_bass_kernel_spmd(nc, in_maps, core_ids=list(range(len(xs))))
    return [r["y"] for r in res.results]


def kernel(**inputs):
    S, NSEQ, NCORES = 2048, 2, 8
    consts = make_consts()
    x = np.ascontiguousarray(inputs["x"], dtype=np.float32).reshape(NCORES, NSEQ * S, D)
    nc, need = build(S, NSEQ, [0, 1, 2, 3])
    ys = _launch(nc, need, [x[c] for c in range(NCORES)], inputs, consts)
    y = np.stack(ys, axis=0)
    return y.reshape(16, S, D).astype(np.float32)
```
